# Optimizing a Trainium2 kernel written in Bass

```python
import jax, jax.numpy as jnp
from jax import lax
import numpy as np

D_MODEL = 2048
BATCH = 4
SEQ = 4096
DEPTH = 1

GRID_W = 64
CTX_LEN = 256
EPS = 1e-6
ATTN_HEADS = 8
ATTN_KV_HEADS = 2
HEAD_DIM = 128
Q_BLOCK = 128
ROPE_BASE = 10000.0
RET_HEADS = 8
RET_DK = 128
RET_DV = 256
RET_CHUNK = 128
PEER_HEADS = 8
PEER_N_KEYS = 128
PEER_N_EXPERTS = PEER_N_KEYS * PEER_N_KEYS
PEER_D_HALF = 128
PEER_TOPK = 16
PEER_BLOCK = 128
ATTN_Q_W = ATTN_HEADS * HEAD_DIM
ATTN_KV_W = ATTN_KV_HEADS * HEAD_DIM
RET_QK_W = RET_HEADS * RET_DK
RET_V_W = RET_HEADS * RET_DV
IN_SPLITS = (ATTN_Q_W, ATTN_KV_W, ATTN_KV_W, RET_QK_W, RET_QK_W, RET_V_W, RET_V_W, D_MODEL, D_MODEL)
IN_WIDTH = ATTN_Q_W + 2 * ATTN_KV_W + 2 * RET_QK_W + 2 * RET_V_W + 2 * D_MODEL

kernel_name = 'hybrid_gqa_retention_peer_block'


def rms_norm(x, w):
    xf = x.astype(jnp.float32)
    y = xf * lax.rsqrt(jnp.mean(xf * xf, axis=-1, keepdims=True) + EPS)
    return (y * w.astype(jnp.float32)).astype(x.dtype)


def modulate(h, shift, scale):
    return h * (1.0 + scale[:, None, :]) + shift[:, None, :]


def axial_rope_tables(length):
    t = jnp.arange(length, dtype=jnp.int32)
    row = (t // GRID_W).astype(jnp.float32)
    col = (t % GRID_W).astype(jnp.float32)
    n_freq = HEAD_DIM // 4
    inv_freq = ROPE_BASE ** (-jnp.arange(n_freq, dtype=jnp.float32) / n_freq)
    ang = jnp.concatenate([row[:, None] * inv_freq, col[:, None] * inv_freq], axis=-1)
    return jnp.cos(ang), jnp.sin(ang)


def apply_rope(x, cos, sin):
    x1, x2 = jnp.split(x, 2, axis=-1)
    cs = cos[None, :, None, :].astype(x.dtype)
    sn = sin[None, :, None, :].astype(x.dtype)
    return jnp.concatenate([x1 * cs - x2 * sn, x2 * cs + x1 * sn], axis=-1)


def split_heads(a, n_heads):
    return a.reshape(a.shape[0], a.shape[1], n_heads, a.shape[2] // n_heads)


def split_projection(p):
    points = np.cumsum(IN_SPLITS)[:-1].tolist()
    return jnp.split(p, points, axis=-1)


def gqa_block(q, k, v):
    b, lq, h, dh = q.shape
    kvh = k.shape[2]
    qg = q.reshape(b, lq, kvh, h // kvh, dh)
    s = jnp.einsum('bqkgd,bskd->bkgqs', qg, k).astype(jnp.float32) * (dh ** -0.5)
    p = jax.nn.softmax(s, axis=-1).astype(v.dtype)
    o = jnp.einsum('bkgqs,bskd->bqkgd', p, v)
    return o.reshape(b, lq, h * dh)


def latent_attention(q, k_all, v_all):
    b, l, h, dh = q.shape
    nb = l // Q_BLOCK
    qb = q.reshape(b, nb, Q_BLOCK, h, dh).transpose(1, 0, 2, 3, 4)
    o = lax.map(lambda qblk: gqa_block(qblk, k_all, v_all), qb)
    return o.transpose(1, 0, 2, 3).reshape(b, l, h * dh)


def retention_chunked(q, k, v, log_gamma, s0):
    b, l, h, _ = q.shape
    dv = v.shape[-1]
    nc = l // RET_CHUNK
    pos = jnp.arange(RET_CHUNK, dtype=jnp.float32)
    diff = pos[:, None] - pos[None, :]
    intra = jnp.where(diff >= 0, jnp.exp(log_gamma[:, None, None] * jnp.maximum(diff, 0.0)), 0.0)
    q_decay = jnp.exp(log_gamma[:, None] * (pos + 1.0)).T
    k_decay = jnp.exp(log_gamma[:, None] * (RET_CHUNK - 1.0 - pos))
    chunk_decay = jnp.exp(log_gamma * RET_CHUNK)

    def to_chunks(a):
        return a.reshape(b, nc, RET_CHUNK, h, a.shape[-1]).transpose(1, 0, 2, 3, 4)

    def step(state, chunk):
        qc, kc, vc = chunk
        s = jnp.einsum('bqhd,bkhd->bhqk', qc, kc) * intra[None]
        inner = jnp.einsum('bhqk,bkhv->bqhv', s, vc)
        cross = jnp.einsum('bqhd,bhdv->bqhv', qc, state) * q_decay[None, :, :, None]
        state = state * chunk_decay[None, :, None, None] + jnp.einsum('bkhd,hk,bkhv->bhdv', kc, k_decay, vc)
        return state, inner + cross

    _, out = lax.scan(step, s0, (to_chunks(q), to_chunks(k), to_chunks(v)))
    return out.transpose(1, 0, 2, 3, 4).reshape(b, l, h, dv)


def retention_final_state(k, v, log_gamma):
    l = k.shape[1]
    w = jnp.exp(log_gamma[:, None] * (l - 1.0 - jnp.arange(l, dtype=jnp.float32)))
    return jnp.einsum('blhd,hl,blhv->bhdv', k, w, v)


def retention_bidir(q, k, v, lg_f, lg_b, s0_f, s0_b):
    o_f = retention_chunked(q, k, v, lg_f, s0_f)
    o_b = retention_chunked(jnp.flip(q, 1), jnp.flip(k, 1), jnp.flip(v, 1), lg_b, s0_b)
    return o_f + jnp.flip(o_b, 1)


def head_norm(o, w):
    mu = jnp.mean(o, axis=-1, keepdims=True)
    var = jnp.mean(jnp.square(o - mu), axis=-1, keepdims=True)
    return (o - mu) * lax.rsqrt(var + EPS) * w.reshape(RET_HEADS, RET_DV).astype(jnp.float32)


def token_mixers(h, hc, cos, sin, w_in, q_norm_w, k_norm_w, ret_decay_fwd, ret_decay_bwd, ret_norm_w,
                 w_attn_branch, w_ret_branch, w_merge_out, update_ctx):
    f32 = jnp.float32
    b, l, _ = h.shape
    qa, ka, va, qr, kr, vr, gr, gate_attn, gate_ret = split_projection(h @ w_in)
    qa_c, ka_c, va_c, qr_c, kr_c, vr_c, gr_c, gate_attn_c, gate_ret_c = split_projection(hc @ w_in)

    q = apply_rope(rms_norm(split_heads(qa, ATTN_HEADS), q_norm_w), cos, sin)
    k = apply_rope(rms_norm(split_heads(ka, ATTN_KV_HEADS), k_norm_w), cos, sin)
    v = split_heads(va, ATTN_KV_HEADS)
    k_c = rms_norm(split_heads(ka_c, ATTN_KV_HEADS), k_norm_w)
    v_c = split_heads(va_c, ATTN_KV_HEADS)
    attn = latent_attention(q, jnp.concatenate([k_c, k], axis=1), jnp.concatenate([v_c, v], axis=1))

    lg_f = -jnp.exp(ret_decay_fwd.astype(f32))
    lg_b = -jnp.exp(ret_decay_bwd.astype(f32))
    rq = apply_rope(split_heads(qr, RET_HEADS), cos, sin).astype(f32)
    rk = apply_rope(split_heads(kr, RET_HEADS), cos, sin).astype(f32) * (RET_DK ** -0.5)
    rv = split_heads(vr, RET_HEADS).astype(f32)
    rk_c = split_heads(kr_c, RET_HEADS).astype(f32) * (RET_DK ** -0.5)
    rv_c = split_heads(vr_c, RET_HEADS).astype(f32)
    s_f = retention_final_state(rk_c, rv_c, lg_f)
    s_b = retention_final_state(jnp.flip(rk_c, 1), jnp.flip(rv_c, 1), lg_b)
    ret = retention_bidir(rq, rk, rv, lg_f, lg_b, s_f, s_b)
    ret = jax.nn.silu(gr) * head_norm(ret, ret_norm_w).reshape(b, l, RET_V_W).astype(h.dtype)

    def merge(attn_o, ret_o, g_attn, g_ret):
        mixed = jax.nn.sigmoid(g_attn) * (attn_o @ w_attn_branch) + jax.nn.sigmoid(g_ret) * (ret_o @ w_ret_branch)
        return mixed @ w_merge_out

    y = merge(attn, ret, gate_attn, gate_ret)
    if not update_ctx:
        return y, None

    lc = hc.shape[1]
    q_c = rms_norm(split_heads(qa_c, ATTN_HEADS), q_norm_w)
    attn_c = gqa_block(q_c, k_c, v_c)
    zeros = jnp.zeros_like(s_f)
    ret_c = retention_bidir(split_heads(qr_c, RET_HEADS).astype(f32), rk_c, rv_c, lg_f, lg_b, zeros, zeros)
    ret_c = jax.nn.silu(gr_c) * head_norm(ret_c, ret_norm_w).reshape(b, lc, RET_V_W).astype(hc.dtype)
    return y, merge(attn_c, ret_c, gate_attn_c, gate_ret_c)


def peer_ffn(h, w_q, sub_keys, u, v):
    b, l, d = h.shape
    nb = (b * l) // PEER_BLOCK
    tokens = h.reshape(nb, PEER_BLOCK, d)

    def one_block(xb):
        q = (xb @ w_q).reshape(PEER_BLOCK, PEER_HEADS, 2, PEER_D_HALF)
        s = jnp.einsum('thpd,hpnd->thpn', q, sub_keys).astype(jnp.float32)
        s1, i1 = lax.top_k(s[:, :, 0], PEER_TOPK)
        s2, i2 = lax.top_k(s[:, :, 1], PEER_TOPK)
        cand_s = (s1[..., :, None] + s2[..., None, :]).reshape(PEER_BLOCK, PEER_HEADS, PEER_TOPK * PEER_TOPK)
        cand_i = (i1[..., :, None] * PEER_N_KEYS + i2[..., None, :]).reshape(PEER_BLOCK, PEER_HEADS, PEER_TOPK * PEER_TOPK)
        top_s, pos = lax.top_k(cand_s, PEER_TOPK)
        expert = jnp.take_along_axis(cand_i, pos, axis=-1)
        g = jax.nn.softmax(top_s, axis=-1)
        a = jnp.einsum('td,thkd->thk', xb, u[expert])
        act = (jax.nn.gelu(a.astype(jnp.float32)) * g).astype(xb.dtype)
        return jnp.einsum('thk,thkd->td', act, v[expert])

    return lax.map(one_block, tokens).reshape(b, l, d)


def setup_inputs(seed: int = 0) -> dict:
    key = jax.random.key(seed)
    ks = jax.random.split(key, 22)
    f32 = jnp.float32

    def nrm(k, shape, scale):
        return jax.random.normal(k, shape, f32) * scale

    def gain(k, shape):
        return 1.0 + 0.02 * jax.random.normal(k, shape, f32)

    gamma = 1.0 - 2.0 ** (-5.0 - jnp.arange(RET_HEADS, dtype=f32))
    z0 = jnp.log(-jnp.log(gamma))
    return {
        'x': nrm(ks[0], (BATCH, SEQ, D_MODEL), 1.0),
        'c': nrm(ks[1], (BATCH, D_MODEL), 1.0),
        'ctx': nrm(ks[2], (BATCH, CTX_LEN, D_MODEL), 1.0),
        'c_ctx': nrm(ks[3], (D_MODEL,), 1.0),
        'ada_w': nrm(ks[4], (DEPTH, D_MODEL, 6 * D_MODEL), 0.5 * D_MODEL ** -0.5),
        'ada_b': nrm(ks[5], (DEPTH, 6 * D_MODEL), 0.02),
        'norm1_w': gain(ks[6], (DEPTH, D_MODEL)),
        'w_in': nrm(ks[7], (DEPTH, D_MODEL, IN_WIDTH), D_MODEL ** -0.5),
        'q_norm_w': gain(ks[8], (DEPTH, HEAD_DIM)),
        'k_norm_w': gain(ks[9], (DEPTH, HEAD_DIM)),
        'ret_decay_fwd': z0 + 0.05 * jax.random.normal(ks[10], (DEPTH, RET_HEADS), f32),
        'ret_decay_bwd': z0 + 0.05 * jax.random.normal(ks[11], (DEPTH, RET_HEADS), f32),
        'ret_norm_w': gain(ks[12], (DEPTH, RET_V_W)),
        'w_attn_branch': nrm(ks[13], (DEPTH, ATTN_Q_W, D_MODEL), ATTN_Q_W ** -0.5),
        'w_ret_branch': nrm(ks[14], (DEPTH, RET_V_W, D_MODEL), RET_V_W ** -0.5),
        'w_merge_out': nrm(ks[15], (DEPTH, D_MODEL, D_MODEL), D_MODEL ** -0.5),
        'norm2_w': gain(ks[16], (DEPTH, D_MODEL)),
        'peer_w_q': nrm(ks[17], (DEPTH, D_MODEL, PEER_HEADS * 2 * PEER_D_HALF), D_MODEL ** -0.5),
        'peer_keys': nrm(ks[18], (DEPTH, PEER_HEADS, 2, PEER_N_KEYS, PEER_D_HALF), PEER_D_HALF ** -0.5),
        'peer_u': nrm(ks[19], (DEPTH, PEER_N_EXPERTS, D_MODEL), D_MODEL ** -0.5),
        'peer_v': nrm(ks[20], (DEPTH, PEER_N_EXPERTS, D_MODEL), 1.0),
        'norm_f_w': gain(ks[21], (D_MODEL,)),
    }


def reference(x, c, ctx, c_ctx, ada_w, ada_b, norm1_w, w_in, q_norm_w, k_norm_w, ret_decay_fwd, ret_decay_bwd,
              ret_norm_w, w_attn_branch, w_ret_branch, w_merge_out, norm2_w, peer_w_q, peer_keys, peer_u, peer_v,
              norm_f_w):
    cos, sin = axial_rope_tables(x.shape[1])
    xc = ctx
    for layer in range(DEPTH):
        update_ctx = layer + 1 < DEPTH
        mod_x = jnp.split(jax.nn.silu(c) @ ada_w[layer] + ada_b[layer], 6, axis=-1)
        mod_c = jnp.split(jax.nn.silu(c_ctx)[None, :] @ ada_w[layer] + ada_b[layer], 6, axis=-1)

        h = modulate(rms_norm(x, norm1_w[layer]), mod_x[0], mod_x[1])
        hc = modulate(rms_norm(xc, norm1_w[layer]), mod_c[0], mod_c[1])
        y, yc = token_mixers(h, hc, cos, sin, w_in[layer], q_norm_w[layer], k_norm_w[layer],
                             ret_decay_fwd[layer], ret_decay_bwd[layer], ret_norm_w[layer],
                             w_attn_branch[layer], w_ret_branch[layer], w_merge_out[layer], update_ctx)
        x = x + mod_x[2][:, None, :] * y

        h2 = modulate(rms_norm(x, norm2_w[layer]), mod_x[3], mod_x[4])
        x = x + mod_x[5][:, None, :] * peer_ffn(h2, peer_w_q[layer], peer_keys[layer], peer_u[layer], peer_v[layer])

        if update_ctx:
            xc = xc + mod_c[2][:, None, :] * yc
            hc2 = modulate(rms_norm(xc, norm2_w[layer]), mod_c[3], mod_c[4])
            xc = xc + mod_c[5][:, None, :] * peer_ffn(hc2, peer_w_q[layer], peer_keys[layer], peer_u[layer], peer_v[layer])
    return rms_norm(x, norm_f_w)
```

```python
import numpy as np
from contextlib import ExitStack
import concourse.bass as bass
import concourse.mybir as mybir
from concourse.bass_utils import run_bass_kernel_spmd

F32 = mybir.dt.float32
BF16 = mybir.dt.bfloat16
I32 = mybir.dt.int32
AF = mybir.ActivationFunctionType
ALU = mybir.AluOpType
AX = mybir.AxisListType

D = 2048
SEQ = 4096
CTX = 256
NT_ALL = 34
NTOK = NT_ALL * 128
OWN0 = 18
NOWN = 16
INW = 11776
EPS = 1e-6
C_QA, C_KA, C_VA, C_QR, C_KR, C_VR, C_GR, C_GA, C_GB = 0, 1024, 1280, 1536, 2560, 3584, 5632, 7680, 9728
SEM_LIMIT = 12000


class Buf:
    def __init__(self, name, accum=False):
        self.name = name
        self.w = {}
        self.r = {}
        self.accum = accum
        self.sem = None
        self.semcnt = 0


class Eng:
    def __init__(self, fw, name, handle):
        self.fw, self.name, self.h = fw, name, handle
        self.n = 0
        self.sems = []
        self.waited = {}
        self.thunks = []

    def cur_token(self):
        e = (self.n - 1) // SEM_LIMIT
        return ((self.name, e), (self.n - 1) % SEM_LIMIT + 1)


class FW:
    def __init__(self, nc, stack):
        self.nc, self.stack = nc, stack
        self.semmap = {}
        self.nsem = 0
        self.dma_owners = []
        self.E = {
            "pe": Eng(self, "pe", nc.tensor), "act": Eng(self, "act", nc.scalar),
            "dve": Eng(self, "dve", nc.vector), "pool": Eng(self, "pool", nc.gpsimd),
            "sp": Eng(self, "sp", nc.sync),
        }

    def newsem(self, key):
        s = self.stack.enter_context(self.nc.semaphore("s%d" % self.nsem))
        self.nsem += 1
        self.semmap[key] = s
        return s

    def _need(self, eng, toks, skip_same=False):
        for key, val in toks.items():
            if key[0] == "pe" and eng.name == "pe":
                continue
            if skip_same and key[0] == eng.name:
                continue
            if eng.waited.get(key, 0) >= val:
                continue
            if key[0] in self.E:
                later = [k for k in eng.waited if k[0] == key[0] and k[1] > key[1]]
                if later:
                    continue
            eng.waited[key] = val
            sem = self.semmap[key]
            eng.thunks.append((lambda h=eng.h, sem=sem, val=val: h.wait_ge(sem, val)))

    def op(self, en, fn, r=(), w=()):
        eng = self.E[en]
        for b in r:
            self._need(eng, b.w)
        for b in w:
            if not b.accum:
                self._need(eng, b.w, skip_same=True)
                self._need(eng, b.r, skip_same=True)
        eng.n += 1
        key, val = eng.cur_token()
        if key not in self.semmap:
            self.newsem(key)
        sem = self.semmap[key]
        eng.thunks.append((lambda fn=fn, sem=sem: fn().then_inc(sem, 1)))
        for b in r:
            b.r[key] = max(b.r.get(key, 0), val)
        for b in w:
            if b.accum:
                b.w[key] = max(b.w.get(key, 0), val)
            else:
                b.w = {key: val}
                b.r = {}

    def dma(self, en, out_ap, in_ap, owner, r=(), w=()):
        eng = self.E[en]
        for b in r:
            self._need(eng, b.w)
        for b in w:
            if not b.accum:
                self._need(eng, b.w)
                self._need(eng, b.r)
        if owner.sem is None:
            owner.sem = ("dma", owner.name)
            self.newsem(owner.sem)
            self.dma_owners.append(owner)
        owner.semcnt += 16
        key, val = owner.sem, owner.semcnt
        sem = self.semmap[key]
        h = eng.h
        eng.thunks.append((lambda h=h, o=out_ap, i=in_ap, sem=sem: h.dma_start(out=o, in_=i).then_inc(sem, 16)))
        for b in r:
            b.r[key] = max(b.r.get(key, 0), val)
        for b in w:
            if b.accum:
                b.w[key] = max(b.w.get(key, 0), val)
            else:
                b.w = {key: val}
                b.r = {}

    def drain(self, en, bufs):
        eng = self.E[en]
        for b in bufs:
            self._need(eng, b.w)
            self._need(eng, b.r)

    def barrier(self):
        toks = {}
        for e in self.E.values():
            if e.n > 0:
                key, val = e.cur_token()
                toks[key] = val
        for o in self.dma_owners:
            toks[o.sem] = o.semcnt
        for e in self.E.values():
            self._need(e, dict(toks))

    def emit(self):
        nc = self.nc
        with nc.Block() as block:
            @block.tensor
            def _(t):
                for th in self.E["pe"].thunks:
                    th()

            @block.scalar
            def _(t):
                for th in self.E["act"].thunks:
                    th()

            @block.vector
            def _(t):
                for th in self.E["dve"].thunks:
                    th()

            @block.gpsimd
            def _(t):
                for th in self.E["pool"].thunks:
                    th()

            @block.sync
            def _(t):
                for th in self.E["sp"].thunks:
                    th()
        for e in self.E.values():
            e.thunks = []


class K:
    def __init__(self, debug=False, stop_after=99):
        self.debug = debug
        self.stop_after = stop_after
        self.stack = ExitStack()
        self.nc = bass.Bass("TRN2", target_bir_lowering=False)
        self.fw = FW(self.nc, self.stack)
        self.nbuf = 0

    def din(self, name, shape, dt=F32):
        return self.nc.dram_tensor(name, list(shape), dt, kind="ExternalInput").ap()

    def dscr(self, name, shape, dt, out=False):
        kind = "ExternalOutput" if (out or (self.debug and (self.debug is True or name in self.debug))) else "Internal"
        ap = self.nc.dram_tensor(name, list(shape), dt, kind=kind).ap()
        return ap, Buf(name, accum=True)

    def sb(self, name, shape, dt):
        t = self.stack.enter_context(self.nc.sbuf_tensor("sb_" + name, list(shape), dt))
        return t, Buf(name)

    def ps(self, name, shape, dt):
        t = self.stack.enter_context(self.nc.psum_tensor(name, list(shape), dt))
        return t, Buf(name)

    def mm(self, out, lhsT, rhs, start, stop, r, w):
        nc = self.nc
        self.fw.op("pe", lambda: nc.tensor.matmul(out, lhsT, rhs, start=start, stop=stop), r=r, w=w)

    def tr(self, out, in_, ident, r, w):
        nc = self.nc
        self.fw.op("pe", lambda: nc.tensor.transpose(out, in_, ident), r=r, w=w)

    def act(self, out, in_, func, r, w, bias=None, scale=None, accum_out=None):
        nc = self.nc
        kw = {}
        if bias is not None:
            kw["bias"] = bias
        if scale is not None:
            kw["scale"] = scale
        if accum_out is not None:
            kw["accum_out"] = accum_out
        self.fw.op("act", lambda: nc.scalar.activation(out=out, in_=in_, func=func, **kw), r=r, w=w)

    def tt(self, out, in0, in1, op, r, w, eng="dve"):
        h = self.nc.vector if eng == "dve" else self.nc.gpsimd
        self.fw.op(eng, lambda: h.tensor_tensor(out=out, in0=in0, in1=in1, op=op), r=r, w=w)

    def ts(self, out, in0, s1, s2, op0, op1, r, w, eng="dve"):
        h = self.nc.vector if eng == "dve" else self.nc.gpsimd
        if s2 is None:
            self.fw.op(eng, lambda: h.tensor_scalar(out=out, in0=in0, scalar1=s1, scalar2=None, op0=op0), r=r, w=w)
        else:
            self.fw.op(eng, lambda: h.tensor_scalar(out=out, in0=in0, scalar1=s1, scalar2=s2, op0=op0, op1=op1), r=r, w=w)

    def stt(self, out, in0, scalar, in1, op0, op1, r, w):
        nc = self.nc
        self.fw.op("dve", lambda: nc.vector.scalar_tensor_tensor(out=out, in0=in0, scalar=scalar, in1=in1, op0=op0, op1=op1), r=r, w=w)

    def red(self, out, in_, op, r, w):
        nc = self.nc
        self.fw.op("dve", lambda: nc.vector.tensor_reduce(out=out, in_=in_, axis=AX.X, op=op), r=r, w=w)

    def recip(self, out, in_, r, w):
        nc = self.nc
        self.fw.op("dve", lambda: nc.vector.reciprocal(out=out, in_=in_), r=r, w=w)

    def ld(self, out_ap, in_ap, owner, r=(), w=(), q="sp"):
        self.fw.dma(q, out_ap, in_ap, owner, r=r, w=w)

    def ldc(self, out_ap, in_ap, owner, r=(), w=()):
        self.fw.dma("pool", out_ap, in_ap, owner, r=r, w=w)


def build(debug=False, stop_after=99):
    k = K(debug=debug, stop_after=stop_after)
    nc, fw = k.nc, k.fw
    tok = k.din("tok", [NTOK, D])
    cT_in = k.din("cT", [128, 16, 2])
    ada_w = k.din("ada_w", [D, 6 * D])
    ada_bT = k.din("ada_bT", [128, 96])
    n1w_in = k.din("n1w", [128, 16])
    n2w_in = k.din("n2w", [128, 16])
    nfw_in = k.din("nfw", [D])
    w_in = k.din("w_in", [D, INW])
    qkw_in = k.din("qkw", [128, 2])
    zdec_in = k.din("zdec", [16])
    rnw_in = k.din("rnw", [D])
    w_ab = k.din("w_ab", [1024, D])
    w_rb = k.din("w_rb", [D, D])
    w_mo = k.din("w_mo", [D, D])
    pwq = k.din("pwq", [D, D])
    keysT_in = k.din("keysT", [128, 16, 128])
    uT = k.din("uT", [D, 128, 128])
    vP = k.din("vP", [128, 128, D])
    cosT_in = k.din("cosT", [128, NTOK])
    sinS_in = k.din("sinS", [128, NTOK])
    cmat_in = k.din("cmat", [128, 8, 128])
    kpos_in = k.din("kpos", [128, 16])
    dist_in = k.din("dist", [128, 2, NT_ALL])
    npos_in = k.din("npos", [128, 2, 16])
    qpos_in = k.din("qpos", [128, 2, NOWN * 128])
    iota_in = k.din("iotas", [128, 512])
    out_d = nc.dram_tensor("out", [NOWN * 128, D], F32, kind="ExternalOutput").ap()
    out_b = Buf("out", accum=True)

    hT_d, hT_b = k.dscr("hT_d", [128, 16, NTOK], BF16)
    gsc_d, gsc_b = k.dscr("gsc_d", [2, D], F32)
    attnT_d, attnT_b = k.dscr("attnT_d", [128, 8, NOWN * 128], BF16)
    retT_d, retT_b = k.dscr("retT_d", [128, 16, NOWN * 128], BF16)
    mixT_d, mixT_b = k.dscr("mixT_d", [128, 16, NOWN * 128], BF16)
    x1_d, x1_b = k.dscr("x1_d", [NOWN * 128, D], F32)
    h2T_d, h2T_b = k.dscr("h2T_d", [128, 16, NOWN * 128], BF16)
    W_d, W_b = k.dscr("W_d", [128, 128, NOWN * 128], BF16)
    ga_d, ga_b = k.dscr("ga_d", [128, 128, NOWN * 128], BF16)
    qpT_d, qpTd_b = k.dscr("qpT_d", [128, 16, NOWN * 128], BF16)
    dbgb = []

    cmat, cmat_b = k.sb("cmat", [128, 8, 128], F32)
    k.ld(cmat[:], cmat_in, cmat_b, w=[cmat_b])
    cbf, cbf_b = k.sb("cbf", [128, 3, 128], BF16)
    k.ldc(cbf[:, 0:2, :], cmat_in[:, 0:2, :], cbf_b, w=[cbf_b])
    fw.op("dve", lambda: nc.vector.memset(cbf[:, 2, :], 1.0), w=[cbf_b])
    ident = cbf[:, 0, :]
    pswap = cbf[:, 1, :]
    ones = cbf[:, 2, :]
    vecs, vecs_b = k.sb("vecs", [128, 10, 16], F32)
    k.ld(vecs[:, 0, :], n1w_in, vecs_b, w=[vecs_b])
    k.ld(vecs[:, 1, :], n2w_in, vecs_b, w=[vecs_b])
    N1W, N2W, G1, G1C, SH1, SH1C, G2, SH2, TMPA, TMPB = range(10)
    mods, mods_b = k.sb("mods", [128, 96, 2], F32)
    psb = [k.ps("ps%d" % i, [128, 512], F32) for i in range(8)]

    def sbt(ph, name, shape, dt):
        return ph.enter_context(nc.sbuf_tensor("sb_" + name, list(shape), dt)), Buf(name)

    def end_phase():
        fw.barrier()
        fw.emit()

    def finish():
        allb = [out_b, hT_b, gsc_b, attnT_b, retT_b, mixT_b, x1_b, h2T_b, W_b, ga_b, qpTd_b] + dbgb
        fw.drain("sp", allb)
        fw.barrier()
        fw.emit()
        return nc

    def memset(ap, val, w, eng="dve"):
        h = nc.vector if eng == "dve" else nc.gpsimd
        fw.op(eng, lambda: h.memset(ap, val), w=w)

    def tcopy(out, in_, r, w, eng="dve"):
        h = nc.vector if eng == "dve" else nc.gpsimd
        fw.op(eng, lambda: h.tensor_copy(out=out, in_=in_), r=r, w=w)

    cT, cT_b = k.sb("cTs", [128, 16, 2], F32)
    k.ld(cT[:], cT_in, cT_b, w=[cT_b])
    sc, sc_b = k.sb("sc", [128, 16, 2], BF16)
    k.act(sc[:], cT[:], AF.Silu, r=[cT_b], w=[sc_b])
    abT, abT_b = k.sb("abT", [128, 96], F32)
    k.ld(abT[:], ada_bT, abT_b, w=[abT_b])
    pm, pm_b = psb[7]

    ada_stack = ExitStack()
    wblk = [sbt(ada_stack, "adaw%d" % i, [128, 16, 1024], BF16) for i in range(2)]

    def ada_load(blk, buf=None):
        if blk >= 12:
            return
        wt, wb = buf if buf is not None else wblk[blk % 2]
        k.ldc(wt[:], ada_w[:, blk * 1024:(blk + 1) * 1024].rearrange("(c p) n -> p c n", p=128), wb, w=[wb])

    def ada_mm(blk, buf=None):
        wt, wb = buf if buf is not None else wblk[blk % 2]
        for j in range(8):
            col = (blk * 8 + j) * 2
            for c in range(16):
                k.mm(pm[:, col:col + 2], wt[:, c, j * 128:(j + 1) * 128], sc[:, c, :], c == 0, c == 15,
                     r=[wb, sc_b], w=[pm_b])

    def ada_mods(c0, c1):
        k.tt(mods[:, c0:c1, :], pm[:, 2 * c0:2 * c1].rearrange("p (a s) -> p a s", s=2),
             abT[:, c0:c1].unsqueeze(2).to_broadcast([128, c1 - c0, 2]), ALU.add, r=[pm_b, abT_b], w=[mods_b])

    def plus1(dst, src):
        k.ts(vecs[:, dst, :], src, 1.0, None, ALU.add, None, r=[mods_b], w=[vecs_b])

    def cpv(dst, src):
        k.ts(vecs[:, dst, :], src, 0.0, None, ALU.add, None, r=[mods_b], w=[vecs_b])

    def ada_finish_B():
        ada_mods(32, 96)
        plus1(TMPA, mods[:, 64:80, 0])
        k.tt(vecs[:, G2, :], vecs[:, TMPA, :], vecs[:, N2W, :], ALU.mult, r=[vecs_b], w=[vecs_b])
        cpv(SH2, mods[:, 48:64, 0])
        cpv(TMPA, mods[:, 32:48, 0])
        cpv(TMPB, mods[:, 80:96, 0])
        gst_b = Buf("gst")
        for gi, slot in ((0, TMPA), (1, TMPB)):
            dst = gsc_d[gi].rearrange("(c p) -> p c", p=128)
            src = vecs[:, slot, :]
            eng = fw.E["sp"]
            fw._need(eng, vecs_b.w)
            if gst_b.sem is None:
                gst_b.sem = ("dma", "gst")
                fw.newsem(gst_b.sem)
                fw.dma_owners.append(gst_b)
            gst_b.semcnt += 16
            sem = fw.semmap[gst_b.sem]
            eng.thunks.append(lambda dst=dst, src=src, sem=sem: nc.sync.dma_start(
                out=dst, in_=src, allow_slow_non_contiguous=True).then_inc(sem, 16))
            gsc_b.w[gst_b.sem] = gst_b.semcnt
            vecs_b.r[gst_b.sem] = gst_b.semcnt

    with ExitStack() as ph:
        ada_load(0)
        for blk in range(4):
            if blk < 3:
                ada_load(blk + 1)
            ada_mm(blk)
        ada_mods(0, 32)
        plus1(TMPA, mods[:, 16:32, 0])
        k.tt(vecs[:, G1, :], vecs[:, TMPA, :], vecs[:, N1W, :], ALU.mult, r=[vecs_b], w=[vecs_b])
        plus1(TMPA, mods[:, 16:32, 1])
        k.tt(vecs[:, G1C, :], vecs[:, TMPA, :], vecs[:, N1W, :], ALU.mult, r=[vecs_b], w=[vecs_b])
        cpv(SH1, mods[:, 0:16, 0])
        cpv(SH1C, mods[:, 0:16, 1])
        end_phase()
    ada_stack.close()

    def norm_to_T(ph_name, ntiles, load_tile, gsel, blkinfo, store_blk, hook=None):
        with ExitStack() as ph:
            xs = [sbt(ph, ph_name + "xs%d" % i, [128, D], BF16) for i in range(2)]
            junk, junk_b = sbt(ph, ph_name + "junk", [128, D], BF16)
            st = [sbt(ph, ph_name + "st%d" % i, [128, 2], F32) for i in range(2)]
            hblk = [sbt(ph, ph_name + "hblk%d" % i, [128, 16, 512], BF16) for i in range(2)]
            hblk = [(t_, (b_, Buf(b_.name + "D"))) for (t_, b_) in hblk]
            ctx_ = {"ph": ph}
            def stats(t):
                x_t, x_b = load_tile(ctx_, t)
                xs_t, xs_b = xs[t % 2]
                s_t, s_b = st[t % 2]
                k.act(junk[:], x_t[:], AF.Square, r=[x_b, s_b], w=[junk_b, s_b], accum_out=s_t[:, 0:1])
                k.act(s_t[:, 1:2], s_t[:, 0:1], AF.Sqrt, r=[s_b], w=[s_b], scale=1.0 / D, bias=EPS)
                k.recip(s_t[:, 1:2], s_t[:, 1:2], r=[s_b], w=[s_b])
                k.ts(xs_t[:], x_t[:], s_t[:, 1:2], None, ALU.mult, None, r=[x_b, s_b], w=[xs_b])
            stats(0)
            for t in range(ntiles):
                xs_t, xs_b = xs[t % 2]
                blk, pos, bw, b0, last = blkinfo(t)
                h_t, h_b = hblk[blk % 2]
                if t + 1 < ntiles:
                    stats(t + 1)
                pa, pa_b = psb[2 * (t % 2)]
                pb, pb_b = psb[2 * (t % 2) + 1]
                gv, sv = gsel(t)
                for c in range(16):
                    pt_, ptb = (pa, pa_b) if c < 8 else (pb, pb_b)
                    pv = pt_[:].bitcast(BF16)
                    cc = c % 8
                    k.tr(pv[:, cc * 128:(cc + 1) * 128], xs_t[:, c * 128:(c + 1) * 128], ident, r=[xs_b, cbf_b], w=[ptb])
                for c in range(16):
                    pt_, ptb = (pa, pa_b) if c < 8 else (pb, pb_b)
                    pv = pt_[:].bitcast(BF16)
                    cc = c % 8
                    if c < 8:
                        k.act(h_t[:, c, pos * 128:(pos + 1) * 128], pv[:, cc * 128:(cc + 1) * 128], AF.Identity,
                              r=[ptb, vecs_b], w=[h_b[0]], scale=vecs[:, gv, c:c + 1], bias=vecs[:, sv, c:c + 1])
                    else:
                        k.ts(h_t[:, c, pos * 128:(pos + 1) * 128], pv[:, cc * 128:(cc + 1) * 128], vecs[:, gv, c:c + 1],
                             vecs[:, sv, c:c + 1], ALU.mult, ALU.add, r=[ptb, vecs_b], w=[h_b[1]])
                if last:
                    store_blk(h_t, h_b, b0, bw)
                if hook is not None:
                    hook(ctx_, t)
            end_phase()

    def p1_blkinfo(t):
        if t < 2:
            return 0, t, 256, 0, t == 1
        blk, pos = 1 + (t - 2) // 4, (t - 2) % 4
        return blk, pos, 512, 256 + (blk - 1) * 512, pos == 3

    def p1_load(ctx_, t):
        if "xt" not in ctx_:
            ctx_["xt"] = [sbt(ctx_["ph"], "p1xt%d" % i, [128, D], F32) for i in range(3)]
        x_t, x_b = ctx_["xt"][t % 3]
        k.ld(x_t[:], tok[t * 128:(t + 1) * 128, :], x_b, w=[x_b])
        return x_t, x_b

    def p1_hook(ctx_, t):
        if t % 4 == 1 and t // 4 < 8:
            blk = 4 + t // 4
            ada_load(blk + 1)
            ada_mm(blk)
            if blk == 11:
                ada_finish_B()
                if debug:
                    dv, dv_b = k.dscr("dbg_vecs", [128, 10, 16], F32)
                    k.ld(dv, vecs[:], vecs_b, r=[vecs_b], w=[dv_b])
                    dbgb.append(dv_b)

    norm_to_T("p1", NT_ALL, p1_load, lambda t: (G1C, SH1C) if t < 2 else (G1, SH1), p1_blkinfo,
              lambda h_t, h_b, b0, bw: k.ld(hT_d[:, :, b0:b0 + bw], h_t[:, :, 0:bw], h_b[0], r=list(h_b), w=[hT_b]), hook=None)
    if stop_after <= 1:
        return finish()

    class QK:
        def __init__(self, ph, name, norm_scratch=True):
            self.kw = [sbt(ph, name + "kw%d" % i, [128, 512], BF16) for i in range(2)]
            if norm_scratch:
                self.sq = [sbt(ph, name + "sq%d" % i, [128, 512], BF16) for i in range(2)]
                self.rs = [sbt(ph, name + "rs%d" % i, [128, 512], F32) for i in range(2)]
            self.t1 = [sbt(ph, name + "t1%d" % i, [128, 512], F32) for i in range(2)]
            self.t2 = [sbt(ph, name + "t2%d" % i, [128, 512], F32) for i in range(2)]
            self.i = 0

        def pre(self, praw, praw_b, bw, scale, norm):
            i = self.i
            self.i ^= 1
            kw, kw_b = self.kw[i]
            k.act(kw[:, 0:bw], praw[:, 0:bw], AF.Copy, r=[praw_b], w=[kw_b], scale=scale)
            if norm:
                sq, sq_b = self.sq[i]
                k.act(sq[:, 0:bw], praw[:, 0:bw], AF.Square, r=[praw_b], w=[sq_b])
            return i

        def post(self, i, bw, norm, cs, cs_b, out_ap, out_b, pss, prot):
            kw, kw_b = self.kw[i]
            t1, t1_b = self.t1[i]
            t2, t2_b = self.t2[i]
            if norm:
                sq, sq_b = self.sq[i]
                rs, rs_b = self.rs[i]
                k.mm(pss[0][:, 0:bw], ones, sq[:, 0:bw], True, True, r=[sq_b, cbf_b], w=[pss[1]])
                k.act(rs[:, 0:bw], pss[0][:, 0:bw], AF.Sqrt, r=[pss[1]], w=[rs_b], bias=128.0 * EPS)
                k.recip(rs[:, 0:bw], rs[:, 0:bw], r=[rs_b], w=[rs_b])
            k.mm(prot[0][:, 0:bw], pswap, kw[:, 0:bw], True, True, r=[kw_b, cbf_b], w=[prot[1]])
            k.tt(t1[:, 0:bw], kw[:, 0:bw], cs[:, 0, 0:bw], ALU.mult, r=[kw_b, cs_b], w=[t1_b])
            k.tt(t2[:, 0:bw], prot[0][:, 0:bw], cs[:, 1, 0:bw], ALU.mult, r=[prot[1], cs_b], w=[t2_b])
            if norm:
                k.tt(t1[:, 0:bw], t1[:, 0:bw], t2[:, 0:bw], ALU.add, r=[t1_b, t2_b], w=[t1_b])
                k.tt(out_ap, t1[:, 0:bw], rs[:, 0:bw], ALU.mult, r=[t1_b, rs_b], w=[out_b])
            else:
                k.tt(out_ap, t1[:, 0:bw], t2[:, 0:bw], ALU.add, r=[t1_b, t2_b], w=[out_b])

        def run(self, praw, praw_b, bw, scale, norm, cs, cs_b, out_ap, out_b, pss, prot):
            i = self.pre(praw, praw_b, bw, scale, norm)
            self.post(i, bw, norm, cs, cs_b, out_ap, out_b, pss, prot)

    def wslice(c0, n):
        return w_in[:, c0:c0 + n].rearrange("(c p) n -> p c n", p=128)

    def blk_range(blk):
        if blk == 0:
            return 0, 256
        return 256 + (blk - 1) * 512, 512

    with ExitStack() as ph:
        kT_all, kT_b = sbt(ph, "kT_all", [128, 2, NTOK], BF16)
        V_all, V_b = sbt(ph, "V_all", [128, NT_ALL, 256], BF16)
        qkw, qkw_b = sbt(ph, "qkw", [128, 2], F32)
        k.ld(qkw[:], qkw_in, qkw_b, w=[qkw_b])
        k.ts(qkw[:], qkw[:], float(np.sqrt(128.0)), None, ALU.mult, None, r=[qkw_b], w=[qkw_b])
        Wk, Wk_b = sbt(ph, "Wk", [128, 16, 256], BF16)
        Wv, Wv_b = sbt(ph, "Wv", [128, 16, 256], BF16)
        Wq, Wq_b = sbt(ph, "Wq", [128, 16, 1024], BF16)
        k.ldc(Wk[:], wslice(C_KA, 256), Wk_b, w=[Wk_b])
        k.ldc(Wv[:], wslice(C_VA, 256), Wv_b, w=[Wv_b])
        k.ldc(Wq[:], wslice(C_QA, 1024), Wq_b, w=[Wq_b])
        hb = [sbt(ph, "a_hb%d" % i, [128, 16, 512], BF16) for i in range(2)]
        cs = [sbt(ph, "a_cs%d" % i, [128, 2, 512], F32) for i in range(2)]
        qk = QK(ph, "a_")
        li = 0
        adaB = sbt(ph, "adawB", [128, 16, 1024], BF16)
        for blk in range(9):
            b0, bw = blk_range(blk)
            h_t, h_b = hb[li % 2]
            c_t, c_b = cs[li % 2]
            li += 1
            k.ld(h_t[:, :, 0:bw], hT_d[:, :, b0:b0 + bw], h_b, r=[hT_b], w=[h_b])
            k.ld(c_t[:, 0, 0:bw], cosT_in[:, b0:b0 + bw], c_b, w=[c_b])
            k.ld(c_t[:, 1, 0:bw], sinS_in[:, b0:b0 + bw], c_b, w=[c_b])
            if blk < 8:
                ada_load(4 + blk, adaB)
            iks = []
            for kvh in range(2):
                praw, praw_b = psb[0] if kvh == 0 else psb[5]
                for c in range(16):
                    k.mm(praw[:, 0:bw], Wk[:, c, kvh * 128:(kvh + 1) * 128], h_t[:, c, 0:bw], c == 0, c == 15,
                         r=[Wk_b, h_b], w=[praw_b])
                iks.append(qk.pre(praw, praw_b, bw, qkw[:, 1:2], True))
            for tl in range(bw // 128):
                t = b0 // 128 + tl
                pv, pv_b = psb[3 + (t % 2)]
                for c in range(16):
                    k.mm(pv[:, 0:256], h_t[:, c, tl * 128:(tl + 1) * 128], Wv[:, c, :], c == 0, c == 15,
                         r=[Wv_b, h_b], w=[pv_b])
                k.act(V_all[:, t, :], pv[:, 0:256], AF.Copy, r=[pv_b], w=[V_b])
            for kvh in range(2):
                qk.post(iks[kvh], bw, True, c_t, c_b, kT_all[:, kvh, b0:b0 + bw], kT_b, psb[1], psb[2])
            if blk < 8:
                ada_mm(4 + blk, adaB)
                if blk == 7:
                    ada_finish_B()
        if debug:
            d1, d1_b = k.dscr("dbg_kT", [128, 2, NTOK], BF16)
            k.ld(d1, kT_all[:], kT_b, r=[kT_b], w=[d1_b])
            d2, d2_b = k.dscr("dbg_V", [128, NT_ALL, 256], BF16)
            k.ld(d2, V_all[:], V_b, r=[V_b], w=[d2_b])
            dbgb.extend([d1_b, d2_b])
        if stop_after <= 2:
            end_phase()
            return finish()
        qblk, qblk_b = sbt(ph, "qblk", [128, 4, 512], BF16)
        ptb_ = [sbt(ph, "pt%d" % i, [128, 512], BF16) for i in range(3)]
        rden, rden_b = sbt(ph, "rden", [128, 512], F32)
        astg = [sbt(ph, "astg%d" % i, [128, 8, 512], BF16) for i in range(2)]
        sc_att = float(128.0 ** -0.5)
        for ob in range(4):
            b0 = 256 + 2048 + ob * 512
            h_t, h_b = hb[li % 2]
            c_t, c_b = cs[li % 2]
            li += 1
            k.ld(h_t[:], hT_d[:, :, b0:b0 + 512], h_b, r=[hT_b], w=[h_b])
            k.ld(c_t[:, 0, :], cosT_in[:, b0:b0 + 512], c_b, w=[c_b])
            k.ld(c_t[:, 1, :], sinS_in[:, b0:b0 + 512], c_b, w=[c_b])
            as_t, as_b = astg[ob % 2]
            for kvh in range(2):
                prev = None
                for g in range(4):
                    hd = kvh * 4 + g
                    praw, praw_b = psb[0] if g % 2 == 0 else psb[7]
                    for c in range(16):
                        k.mm(praw[:], Wq[:, c, hd * 128:(hd + 1) * 128], h_t[:, c, :], c == 0, c == 15,
                             r=[Wq_b, h_b], w=[praw_b])
                    iq = qk.pre(praw, praw_b, 512, qkw[:, 0:1], True)
                    if prev is not None:
                        qk.post(prev[0], 512, True, c_t, c_b, qblk[:, prev[1], :], qblk_b, psb[1], psb[2])
                    prev = (iq, g)
                qk.post(prev[0], 512, True, c_t, c_b, qblk[:, prev[1], :], qblk_b, psb[1], psb[2])
                for tl in range(4):
                    qsel = qblk[:, :, tl * 128:(tl + 1) * 128]
                    po, po_b = psb[5]
                    pd, pd_b = psb[6]
                    ring = [psb[3], psb[4], psb[7]]

                    def emit_st(kt, kvh=kvh, qsel=qsel, ring=ring):
                        pst, pst_b = ring[kt % 3]
                        p_t, p_b = ptb_[kt % 3]
                        k.mm(pst[:].rearrange("p (g q) -> p g q", g=4), kT_all[:, kvh, kt * 128:(kt + 1) * 128], qsel,
                             True, True, r=[kT_b, qblk_b], w=[pst_b])
                        k.act(p_t[:], pst[:], AF.Exp, r=[pst_b], w=[p_b], scale=sc_att)
                    emit_st(0)
                    emit_st(1)
                    for kt in range(NT_ALL):
                        if kt + 2 < NT_ALL:
                            emit_st(kt + 2)
                        p_t, p_b = ptb_[kt % 3]
                        k.mm(po[:], V_all[:, kt, kvh * 128:(kvh + 1) * 128], p_t[:], kt == 0, kt == NT_ALL - 1,
                             r=[V_b, p_b], w=[po_b])
                        k.mm(pd[:], ones, p_t[:], kt == 0, kt == NT_ALL - 1, r=[cbf_b, p_b], w=[pd_b])
                    k.recip(rden[:], pd[:], r=[pd_b], w=[rden_b])
                    k.tt(as_t[:, kvh * 4:(kvh + 1) * 4, tl * 128:(tl + 1) * 128],
                         po[:].rearrange("p (g q) -> p g q", g=4), rden[:].rearrange("p (g q) -> p g q", g=4),
                         ALU.mult, r=[po_b, rden_b], w=[as_b])
            k.ld(attnT_d[:, :, ob * 512:(ob + 1) * 512], as_t[:], as_b, r=[as_b], w=[attnT_b])
        end_phase()
    if stop_after <= 3:
        return finish()

    with ExitStack() as ph:
        lg, lg_b = sbt(ph, "lg", [128, 16], F32)
        k.ld(lg[:], zdec_in.partition_broadcast(128), lg_b, w=[lg_b])
        k.act(lg[:], lg[:], AF.Exp, r=[lg_b], w=[lg_b])
        k.ts(lg[:], lg[:], -1.0, None, ALU.mult, None, r=[lg_b], w=[lg_b])
        kpos, kpos_b = sbt(ph, "kpos", [128, 16], F32)
        k.ld(kpos[:], kpos_in, kpos_b, w=[kpos_b])
        dist, dist_b = sbt(ph, "dist", [128, 2, NT_ALL], F32)
        k.ld(dist[:], dist_in, dist_b, w=[dist_b])
        Dk, Dk_b = sbt(ph, "Dk", [128, 16], F32)
        k.tt(Dk[:], kpos[:], lg[:], ALU.mult, r=[kpos_b, lg_b], w=[Dk_b])
        k.act(Dk[:], Dk[:], AF.Exp, r=[Dk_b], w=[Dk_b])
        npos, npos_b = sbt(ph, "npos", [128, 2, 16], F32)
        k.ld(npos[:], npos_in, npos_b, w=[npos_b])
        Kdec, Kdec_b = sbt(ph, "Kdec", [128, 2, 16], F32)
        qp_t, qp_b = sbt(ph, "qp_t", [128, 2, 512], F32)
        qsc, qsc_b = sbt(ph, "qsc", [128, 2, 512], F32)
        MT, MT_b = sbt(ph, "MT", [128, 8, 128], F32)
        e1, e1_b = sbt(ph, "e1", [128, 128], F32)
        e2, e2_b = sbt(ph, "e2", [128, 128], F32)
        for h in range(8):
            k.act(e1[:], cmat[:, 2, :], AF.Exp, r=[cmat_b, lg_b], w=[e1_b], scale=lg[:, h:h + 1])
            k.tt(e1[:], e1[:], cmat[:, 3, :], ALU.mult, r=[e1_b, cmat_b], w=[e1_b])
            k.act(e2[:], cmat[:, 4, :], AF.Exp, r=[cmat_b, lg_b], w=[e2_b], scale=lg[:, 8 + h:9 + h])
            k.tt(e2[:], e2[:], cmat[:, 5, :], ALU.mult, r=[e2_b, cmat_b], w=[e2_b])
            k.tt(MT[:, h, :], e1[:], e2[:], ALU.add, r=[e1_b, e2_b], w=[MT_b])
        rnw, rnw_b = sbt(ph, "rnw", [128, D], F32)
        k.ld(rnw[:], rnw_in.partition_broadcast(128), rnw_b, w=[rnw_b])
        Wq_h, Wq_hb = sbt(ph, "Wq_h", [128, 16, 128], BF16)
        Wk_h, Wk_hb = sbt(ph, "Wk_h", [128, 16, 128], BF16)
        Wv_h, Wv_hb = sbt(ph, "Wv_h", [128, 16, 256], BF16)
        Wg_h, Wg_hb = sbt(ph, "Wg_h", [128, 16, 256], BF16)
        hb = [sbt(ph, "r_hb%d" % i, [128, 16, 512], BF16) for i in range(2)]
        cs = [sbt(ph, "r_cs%d" % i, [128, 2, 512], F32) for i in range(2)]
        qk = QK(ph, "r_", norm_scratch=False)
        ktb = [sbt(ph, "ktb%d" % i, [128, 512], BF16) for i in range(2)]
        v_all, v_b = sbt(ph, "v_all", [128, NT_ALL, 256], BF16)
        kpre, kpre_b = sbt(ph, "kpre", [128, 2, 18, 128], BF16)
        kdf, kdf_b = sbt(ph, "kdf", [128, 16, 128], BF16)
        kdb, kdb_b = sbt(ph, "kdb", [128, 16, 128], BF16)
        qT_own, qT_b = sbt(ph, "qT_own", [128, 2048], BF16)
        qf, qf_b = sbt(ph, "qf", [128, 2048], BF16)
        qbk, qbk_b = sbt(ph, "qbk", [128, 2048], BF16)
        kT_own, kTo_b = sbt(ph, "kT_own", [128, 2048], BF16)
        g_own, g_b = sbt(ph, "g_own", [128, 16, 256], BF16)
        o_acc, o_b = sbt(ph, "o_acc", [128, 16, 256], F32)
        o_bufs = [Buf("o_acc%d" % j) for j in range(16)]
        SbfF = [sbt(ph, "SbfF%d" % i, [128, 256], BF16) for i in range(2)]
        SbfB = [sbt(ph, "SbfB%d" % i, [128, 256], BF16) for i in range(2)]
        AT = [sbt(ph, "AT%d" % i, [128, 128], BF16) for i in range(2)]
        junk2, junk2_b = sbt(ph, "junk2", [128, 256], F32)
        ytmp = [sbt(ph, "ytmp%d" % i, [128, 256], F32) for i in range(2)]
        ybf = [sbt(ph, "ybf%d" % i, [128, 256], BF16) for i in range(4)]
        hst = [sbt(ph, "hst%d" % i, [128, 8], F32) for i in range(2)]
        wpre, wpre_b = sbt(ph, "wpre", [128, 2, NT_ALL], F32)
        rstage, rst_b = sbt(ph, "rstage", [128, 2, 2048], BF16)
        li = 0
        rk_scale = float(128.0 ** -0.5)
        pending_norm = []

        def drain_norm(n):
            for _ in range(n):
                if pending_norm:
                    for f in pending_norm.pop(0):
                        f()
        for h in range(8):
            k.ldc(Wq_h[:], wslice(C_QR + h * 128, 128), Wq_hb, w=[Wq_hb])
            k.ldc(Wk_h[:], wslice(C_KR + h * 128, 128), Wk_hb, w=[Wk_hb])
            k.ldc(Wv_h[:], wslice(C_VR + h * 256, 256), Wv_hb, w=[Wv_hb])
            k.ldc(Wg_h[:], wslice(C_GR + h * 256, 256), Wg_hb, w=[Wg_hb])
            for d in range(2):
                k.act(wpre[:, d, :], dist[:, d, :], AF.Exp, r=[dist_b, lg_b], w=[wpre_b], scale=lg[:, d * 8 + h:d * 8 + h + 1])
                k.act(Kdec[:, d, :], npos[:, d, :], AF.Exp, r=[npos_b, lg_b], w=[Kdec_b], scale=lg[:, d * 8 + h:d * 8 + h + 1])
            for blk in range(9):
                b0, bw = blk_range(blk)
                own = blk >= 5
                ob = blk - 5
                h_t, h_b = hb[li % 2]
                c_t, c_b = cs[li % 2]
                kt_t, kt_b = ktb[li % 2]
                li += 1
                k.ld(h_t[:, :, 0:bw], hT_d[:, :, b0:b0 + bw], h_b, r=[hT_b], w=[h_b])
                k.ld(c_t[:, 0, 0:bw], cosT_in[:, b0:b0 + bw], c_b, w=[c_b])
                k.ld(c_t[:, 1, 0:bw], sinS_in[:, b0:b0 + bw], c_b, w=[c_b])
                praw, praw_b = psb[0]
                for c in range(16):
                    k.mm(praw[:, 0:bw], Wk_h[:, c, :], h_t[:, c, 0:bw], c == 0, c == 15, r=[Wk_hb, h_b], w=[praw_b])
                if own:
                    kout, kout_b = kT_own[:, ob * 512:(ob + 1) * 512], kTo_b
                    kfull = kT_own
                    koff = ob * 512
                else:
                    kout, kout_b = kt_t[:, 0:bw], kt_b
                    kfull = kt_t
                    koff = 0
                ik = qk.pre(praw, praw_b, bw, rk_scale, False)
                ntl = bw // 128
                for tl in range(ntl):
                    t = b0 // 128 + tl
                    pv, pv_b = psb[2 + (t % 2)]
                    for c in range(16):
                        k.mm(pv[:, 0:256], h_t[:, c, tl * 128:(tl + 1) * 128], Wv_h[:, c, :], c == 0, c == 15,
                             r=[Wv_hb, h_b], w=[pv_b])
                    k.act(v_all[:, t, :], pv[:, 0:256], AF.Copy, r=[pv_b], w=[v_b])
                qk.post(ik, bw, False, c_t, c_b, kout, kout_b, None, psb[1])
                if own:
                    praw, praw_b = psb[0]
                    for c in range(16):
                        k.mm(praw[:], Wq_h[:, c, :], h_t[:, c, :], c == 0, c == 15, r=[Wq_hb, h_b], w=[praw_b])
                    qs = slice(ob * 512, (ob + 1) * 512)
                    iq = qk.pre(praw, praw_b, 512, 1.0, False)
                    for tl in range(4):
                        j = ob * 4 + tl
                        pg, pg_b = psb[6 + (tl % 2)]
                        for c in range(16):
                            k.mm(pg[:, 0:256], h_t[:, c, tl * 128:(tl + 1) * 128], Wg_h[:, c, :], c == 0, c == 15,
                                 r=[Wg_hb, h_b], w=[pg_b])
                        k.act(g_own[:, j, :], pg[:, 0:256], AF.Silu, r=[pg_b], w=[g_b])
                    qk.post(iq, 512, False, c_t, c_b, qT_own[:, qs], qT_b, None, psb[1])
                    k.ld(qp_t[:], qpos_in[:, :, qs], qp_b, w=[qp_b])
                    for d in range(2):
                        k.act(qsc[:, d, :], qp_t[:, d, :], AF.Exp, r=[qp_b, lg_b], w=[qsc_b], scale=lg[:, d * 8 + h:d * 8 + h + 1])
                    k.tt(qf[:, qs], qT_own[:, qs], qsc[:, 0, :], ALU.mult, r=[qT_b, qsc_b], w=[qf_b])
                    k.tt(qbk[:, qs], qT_own[:, qs], qsc[:, 1, :], ALU.mult, r=[qT_b, qsc_b], w=[qbk_b])
                for tl in range(ntl):
                    t = b0 // 128 + tl
                    ptr, ptr_b = psb[4 + (t % 2)]
                    ptv = ptr[:].bitcast(BF16)
                    k.tr(ptv[:, 0:128], kfull[:, koff + tl * 128:koff + (tl + 1) * 128], ident, r=[kout_b, cbf_b], w=[ptr_b])
                    if t < 18:
                        for d in range(2):
                            k.act(kpre[:, d, t, :], ptv[:, 0:128], AF.Copy, r=[ptr_b, wpre_b], w=[kpre_b],
                                  scale=wpre[:, d, t:t + 1])
                    else:
                        j = t - 18
                        k.act(kdf[:, j, :], ptv[:, 0:128], AF.Copy, r=[ptr_b, Kdec_b], w=[kdf_b], scale=Kdec[:, 0, j:j + 1])
                        k.act(kdb[:, j, :], ptv[:, 0:128], AF.Copy, r=[ptr_b, Kdec_b], w=[kdb_b], scale=Kdec[:, 1, j:j + 1])
                if blk <= 4:
                    drain_norm(1)
            pSf, pSf_b = psb[6]
            pSb, pSb_b = psb[7]
            for t in range(18):
                k.mm(pSf[:, 0:256], kpre[:, 0, t, :], v_all[:, t, :], t == 0, True, r=[kpre_b, v_b], w=[pSf_b])
                k.mm(pSb[:, 0:256], kpre[:, 1, t, :], v_all[:, t, :], t == 0, True, r=[kpre_b, v_b], w=[pSb_b])
            k.act(SbfF[0][0][:], pSf[:, 0:256], AF.Copy, r=[pSf_b], w=[SbfF[0][1]])
            k.act(SbfB[0][0][:], pSb[:, 0:256], AF.Copy, r=[pSb_b], w=[SbfB[0][1]])
            for s in range(16):
                j = s
                js = slice(j * 128, (j + 1) * 128)
                pst, pst_b = psb[0]
                k.mm(pst[:, 0:128], kT_own[:, js], qT_own[:, js], True, True, r=[kTo_b, qT_b], w=[pst_b])
                a_t, a_b = AT[s % 2]
                k.tt(a_t[:], pst[:, 0:128], MT[:, h, :], ALU.mult, r=[pst_b, MT_b], w=[a_b])
                po, po_b = psb[1 + (s % 2)]
                sf_t, sf_b = SbfF[s % 2]
                k.mm(po[:, 0:256], qf[:, js], sf_t[:], True, False, r=[qf_b, sf_b], w=[po_b])
                k.mm(po[:, 0:256], a_t[:], v_all[:, 18 + j, :], False, True, r=[a_b, v_b], w=[po_b])
                if s < 8:
                    k.act(o_acc[:, j, :], po[:, 0:256], AF.Copy, r=[po_b], w=[o_bufs[j]])
                else:
                    k.tt(o_acc[:, j, :], o_acc[:, j, :], po[:, 0:256], ALU.add, r=[o_bufs[j], po_b], w=[o_bufs[j]])
                if s < 15:
                    k.mm(pSf[:, 0:256], kdf[:, j, :], v_all[:, 18 + j, :], False, True, r=[kdf_b, v_b], w=[pSf_b])
                    nf_t, nf_b = SbfF[(s + 1) % 2]
                    k.act(nf_t[:], pSf[:, 0:256], AF.Copy, r=[pSf_b], w=[nf_b])
                j = 15 - s
                js = slice(j * 128, (j + 1) * 128)
                pc, pc_b = psb[4 + (s % 2)]
                sb_t, sb_b = SbfB[s % 2]
                k.mm(pc[:, 0:256], qbk[:, js], sb_t[:], True, True, r=[qbk_b, sb_b], w=[pc_b])
                if s < 8:
                    k.act(o_acc[:, j, :], pc[:, 0:256], AF.Copy, r=[pc_b], w=[o_bufs[j]])
                else:
                    k.tt(o_acc[:, j, :], o_acc[:, j, :], pc[:, 0:256], ALU.add, r=[o_bufs[j], pc_b], w=[o_bufs[j]])
                if s < 15:
                    k.mm(pSb[:, 0:256], kdb[:, j, :], v_all[:, 18 + j, :], False, True, r=[kdb_b, v_b], w=[pSb_b])
                    nb_t, nb_b = SbfB[(s + 1) % 2]
                    k.act(nb_t[:], pSb[:, 0:256], AF.Copy, r=[pSb_b], w=[nb_b])
            def mk_part1(j0, h=h):
                def part1():
                    jj = (j0, j0 + 1)
                    O = [o_acc[:, j, :] for j in jj]
                    OB = [o_bufs[j] for j in jj]
                    ST_ = [hst[j % 2] for j in jj]
                    YT = [ytmp[j % 2] for j in jj]
                    YB = [ybf[j % 4] for j in jj]
                    for i in range(2):
                        k.act(junk2[:], O[i], AF.Copy, r=[OB[i]], w=[junk2_b, ST_[i][1]], accum_out=ST_[i][0][:, 0:1])
                    for i in range(2):
                        k.act(junk2[:], O[i], AF.Square, r=[OB[i]], w=[junk2_b, ST_[i][1]], accum_out=ST_[i][0][:, 1:2])
                    for i in range(2):
                        s_t, s_b = ST_[i]
                        k.ts(s_t[:, 2:3], s_t[:, 0:1], 1.0 / 256, None, ALU.mult, None, r=[s_b], w=[s_b])
                    for i in range(2):
                        s_t, s_b = ST_[i]
                        k.tt(s_t[:, 3:4], s_t[:, 2:3], s_t[:, 2:3], ALU.mult, r=[s_b], w=[s_b])
                    for i in range(2):
                        s_t, s_b = ST_[i]
                        k.stt(s_t[:, 4:5], s_t[:, 1:2], 1.0 / 256, s_t[:, 3:4], ALU.mult, ALU.subtract, r=[s_b], w=[s_b])
                    for i in range(2):
                        s_t, s_b = ST_[i]
                        k.act(s_t[:, 5:6], s_t[:, 4:5], AF.Sqrt, r=[s_b], w=[s_b], bias=EPS)
                    for i in range(2):
                        s_t, s_b = ST_[i]
                        k.recip(s_t[:, 5:6], s_t[:, 5:6], r=[s_b], w=[s_b])
                    for i in range(2):
                        s_t, s_b = ST_[i]
                        k.ts(YT[i][0][:], O[i], s_t[:, 2:3], s_t[:, 5:6], ALU.subtract, ALU.mult, r=[OB[i], s_b], w=[YT[i][1]])
                    for i in range(2):
                        k.tt(YT[i][0][:], YT[i][0][:], rnw[:, h * 256:(h + 1) * 256], ALU.mult, r=[YT[i][1], rnw_b], w=[YT[i][1]])
                    for i in range(2):
                        k.tt(YB[i][0][:], YT[i][0][:], g_own[:, jj[i], :], ALU.mult, r=[YT[i][1], g_b], w=[YB[i][1]])
                return part1

            def mk_part2(j0):
                def part2():
                    for j in (j0, j0 + 1):
                        yb_t, yb_b = ybf[j % 4]
                        ptr, ptr_b = psb[4 + (j % 2)]
                        ptv = ptr[:].bitcast(BF16)
                        k.tr(ptv[:, 0:128], yb_t[:, 0:128], ident, r=[yb_b, cbf_b], w=[ptr_b])
                        k.tr(ptv[:, 128:256], yb_t[:, 128:256], ident, r=[yb_b, cbf_b], w=[ptr_b])
                        k.act(rstage[:, :, j * 128:(j + 1) * 128], ptv[:, 0:256].rearrange("p (a b) -> p a b", a=2), AF.Copy,
                              r=[ptr_b], w=[rst_b])
                return part2

            def mk_store(h=h):
                def store():
                    k.ld(retT_d[:, 2 * h:2 * h + 2, :], rstage[:], rst_b, r=[rst_b], w=[retT_b])
                return store
            P1 = [mk_part1(j0) for j0 in range(0, 16, 2)]
            P2 = [mk_part2(j0) for j0 in range(0, 16, 2)]
            pending_norm.extend([P1[0:2], P2[0:2] + P1[2:4], P2[2:4] + P1[4:6], P2[4:6] + P1[6:8], P2[6:8] + [mk_store()]])
        drain_norm(len(pending_norm))
        end_phase()
    if stop_after <= 4:
        return finish()

    with ExitStack() as ph:
        Wab_n, Wab_b = sbt(ph, "Wab_n", [128, 8, 512], BF16)
        Wrb_n, Wrb_b = sbt(ph, "Wrb_n", [128, 16, 512], BF16)
        Wga_n, Wga_b = sbt(ph, "Wga_n", [128, 16, 512], BF16)
        Wgb_n, Wgb_b = sbt(ph, "Wgb_n", [128, 16, 512], BF16)
        abk = [sbt(ph, "m_ab%d" % i, [128, 8, 512], BF16) for i in range(2)]
        rbk = [sbt(ph, "m_rb%d" % i, [128, 16, 512], BF16) for i in range(2)]
        hbk = [sbt(ph, "m_hb%d" % i, [128, 16, 512], BF16) for i in range(2)]
        sga = [sbt(ph, "sga%d" % i, [128, 512], F32) for i in range(2)]
        sgb = [sbt(ph, "sgb%d" % i, [128, 512], F32) for i in range(2)]
        m1 = [sbt(ph, "m1%d" % i, [128, 512], F32) for i in range(2)]
        m2 = [sbt(ph, "m2%d" % i, [128, 512], F32) for i in range(2)]
        mixb = [sbt(ph, "mixb%d" % i, [128, 512], BF16) for i in range(2)]
        mstage = [sbt(ph, "mstage%d" % i, [128, 4, 512], BF16) for i in range(2)]
        li = 0
        pend5 = []
        for nb in range(4):
            ns = slice(nb * 512, (nb + 1) * 512)
            k.ldc(Wab_n[:], w_ab[:, ns].rearrange("(c p) n -> p c n", p=128), Wab_b, w=[Wab_b])
            k.ldc(Wrb_n[:], w_rb[:, ns].rearrange("(c p) n -> p c n", p=128), Wrb_b, w=[Wrb_b])
            k.ldc(Wga_n[:], wslice(C_GA + nb * 512, 512), Wga_b, w=[Wga_b])
            k.ldc(Wgb_n[:], wslice(C_GB + nb * 512, 512), Wgb_b, w=[Wgb_b])
            for ob in range(4):
                a_t, a_b = abk[li % 2]
                r_t, r_b = rbk[li % 2]
                h_t, h_b = hbk[li % 2]
                ms_t, ms_b = mstage[li % 2]
                li += 1
                os_ = slice(ob * 512, (ob + 1) * 512)
                k.ld(a_t[:], attnT_d[:, :, os_], a_b, r=[attnT_b], w=[a_b])
                k.ld(r_t[:], retT_d[:, :, os_], r_b, r=[retT_b], w=[r_b])
                k.ld(h_t[:], hT_d[:, :, 2304 + ob * 512:2304 + (ob + 1) * 512], h_b, r=[hT_b], w=[h_b])
                for tl in range(4):
                    ts_ = slice(tl * 128, (tl + 1) * 128)
                    i2 = tl % 2
                    base = 4 * (tl % 2)
                    pGa, pGa_b = psb[base + 0]
                    pGb, pGb_b = psb[base + 1]
                    pA, pA_b = psb[base + 2]
                    pR, pR_b = psb[base + 3]
                    for c in range(16):
                        k.mm(pGa[:], h_t[:, c, ts_], Wga_n[:, c, :], c == 0, c == 15, r=[h_b, Wga_b], w=[pGa_b])
                    k.act(sga[i2][0][:], pGa[:], AF.Sigmoid, r=[pGa_b], w=[sga[i2][1]])
                    for c in range(16):
                        k.mm(pGb[:], h_t[:, c, ts_], Wgb_n[:, c, :], c == 0, c == 15, r=[h_b, Wgb_b], w=[pGb_b])
                    k.act(sgb[i2][0][:], pGb[:], AF.Sigmoid, r=[pGb_b], w=[sgb[i2][1]])
                    for hh in range(8):
                        k.mm(pA[:], a_t[:, hh, ts_], Wab_n[:, hh, :], hh == 0, hh == 7, r=[a_b, Wab_b], w=[pA_b])
                    k.tt(m1[i2][0][:], pA[:], sga[i2][0][:], ALU.mult, r=[pA_b, sga[i2][1]], w=[m1[i2][1]])
                    for c in range(16):
                        k.mm(pR[:], r_t[:, c, ts_], Wrb_n[:, c, :], c == 0, c == 15, r=[r_b, Wrb_b], w=[pR_b])
                    k.tt(m2[i2][0][:], pR[:], sgb[i2][0][:], ALU.mult, r=[pR_b, sgb[i2][1]], w=[m2[i2][1]])
                    k.tt(mixb[i2][0][:], m1[i2][0][:], m2[i2][0][:], ALU.add, r=[m1[i2][1], m2[i2][1]], w=[mixb[i2][1]])
                    def fin(pGa=pGa, pGa_b=pGa_b, i2=i2, ms_t=ms_t, ms_b=ms_b, ts_=ts_, tl=tl, nb=nb, os_=os_):
                        ptv = pGa[:].bitcast(BF16)
                        for cc in range(4):
                            k.tr(ptv[:, cc * 128:(cc + 1) * 128], mixb[i2][0][:, cc * 128:(cc + 1) * 128], ident,
                                 r=[mixb[i2][1], cbf_b], w=[pGa_b])
                        k.act(ms_t[:, :, ts_], ptv[:, 0:512].rearrange("p (a b) -> p a b", a=4), AF.Copy, r=[pGa_b], w=[ms_b])
                        if tl == 3:
                            k.ld(mixT_d[:, nb * 4:(nb + 1) * 4, os_], ms_t[:], ms_b, r=[ms_b], w=[mixT_b])
                    if pend5:
                        pend5.pop()()
                    pend5.append(fin)
        pend5.pop()()
        end_phase()
    if stop_after <= 5:
        return finish()

    def load_grow(ph, nm):
        grow, grow_b = sbt(ph, nm, [128, 3, D], F32)
        k.ld(grow[:, 0, :], gsc_d[0].partition_broadcast(128), grow_b, r=[gsc_b], w=[grow_b])
        k.ld(grow[:, 1, :], gsc_d[1].partition_broadcast(128), grow_b, r=[gsc_b], w=[grow_b])
        k.ld(grow[:, 2, :], nfw_in.partition_broadcast(128), grow_b, w=[grow_b])
        return grow, grow_b

    def p6_load(ctx_, t):
        ph = ctx_["ph"]
        if "Wmo" not in ctx_:
            ctx_["grow"] = load_grow(ph, "grow6")
            ctx_["Wmo"] = sbt(ph, "Wmo", [128, 16, D], BF16)
            for nb in range(4):
                k.ldc(ctx_["Wmo"][0][:, :, nb * 512:(nb + 1) * 512],
                      w_mo[:, nb * 512:(nb + 1) * 512].rearrange("(c p) n -> p c n", p=128), ctx_["Wmo"][1], w=[ctx_["Wmo"][1]])
            ctx_["mb"] = [sbt(ph, "p6mb%d" % i, [128, 16, 512], BF16) for i in range(2)]
            ctx_["xt"] = [sbt(ph, "p6xt%d" % i, [128, D], F32) for i in range(2)]
            ctx_["x1"] = [sbt(ph, "p6x1%d" % i, [128, D], F32) for i in range(2)]
        Wmo, Wmo_b = ctx_["Wmo"]
        grow, grow_b = ctx_["grow"]
        ob, tl = divmod(t, 4)
        m_t, m_b = ctx_["mb"][ob % 2]
        if tl == 0:
            k.ld(m_t[:], mixT_d[:, :, ob * 512:(ob + 1) * 512], m_b, r=[mixT_b], w=[m_b])
        x_t, x_b = ctx_["xt"][t % 2]
        x1_t, x1t_b = ctx_["x1"][t % 2]
        k.ld(x_t[:], tok[(OWN0 + t) * 128:(OWN0 + t + 1) * 128, :], x_b, w=[x_b])
        for nb in range(4):
            ns = slice(nb * 512, (nb + 1) * 512)
            py, py_b = psb[4 + nb]
            for c in range(16):
                k.mm(py[:], m_t[:, c, tl * 128:(tl + 1) * 128], Wmo[:, c, ns], c == 0, c == 15, r=[m_b, Wmo_b], w=[py_b])
            k.tt(x1_t[:, ns], py[:], grow[:, 0, ns], ALU.mult, r=[py_b, grow_b], w=[x1t_b])
            k.tt(x1_t[:, ns], x1_t[:, ns], x_t[:, ns], ALU.add, r=[x1t_b, x_b], w=[x1t_b])
        k.ld(x1_d[t * 128:(t + 1) * 128, :], x1_t[:], x1t_b, r=[x1t_b], w=[x1_b])
        return x1_t, x1t_b

    norm_to_T("p6", NOWN, p6_load, lambda t: (G2, SH2), lambda t: (t // 4, t % 4, 512, (t // 4) * 512, t % 4 == 3),
              lambda h_t, h_b, b0, bw: k.ld(h2T_d[:, :, b0:b0 + bw], h_t[:, :, 0:bw], h_b[0], r=list(h_b), w=[h2T_b]))
    if stop_after <= 6:
        return finish()

    with ExitStack() as ph:
        Wpq, Wpq_b = sbt(ph, "Wpq", [128, 16, D], BF16)
        for nb in range(4):
            k.ldc(Wpq[:, :, nb * 512:(nb + 1) * 512], pwq[:, nb * 512:(nb + 1) * 512].rearrange("(c p) n -> p c n", p=128),
                  Wpq_b, w=[Wpq_b])
        h2a = [sbt(ph, "p7ah%d" % i, [128, 16, 512], BF16) for i in range(2)]
        qst = [sbt(ph, "p7aq%d" % i, [128, 16, 512], BF16) for i in range(2)]
        for ob in range(4):
            h_t, h_b = h2a[ob % 2]
            q_t, q_b = qst[ob % 2]
            k.ld(h_t[:], h2T_d[:, :, ob * 512:(ob + 1) * 512], h_b, r=[h2T_b], w=[h_b])
            for hp in range(16):
                pq, pq_b = psb[hp % 4]
                for c in range(16):
                    k.mm(pq[:], Wpq[:, c, hp * 128:(hp + 1) * 128], h_t[:, c, :], c == 0, c == 15, r=[Wpq_b, h_b], w=[pq_b])
                if hp % 2 == 0:
                    k.act(q_t[:, hp, :], pq[:], AF.Copy, r=[pq_b], w=[q_b])
                else:
                    tcopy(q_t[:, hp, :], pq[:], r=[pq_b], w=[q_b])
            k.ld(qpT_d[:, :, ob * 512:(ob + 1) * 512], q_t[:], q_b, r=[q_b], w=[qpTd_b])
        end_phase()

    with ExitStack() as ph:
        keys, keys_b = sbt(ph, "keys", [128, 16, 128], BF16)
        k.ldc(keys[:], keysT_in, keys_b, w=[keys_b])
        iot, iot_b = sbt(ph, "iot", [128, 512], F32)
        k.ld(iot[:], iota_in, iot_b, w=[iot_b])
        iof = iot[:, 0:128]
        io256 = iot[:, 128:384].bitcast(I32)
        io128 = iot[:, 384:512].bitcast(I32)
        qpT, qpT_b = sbt(ph, "qpT", [128, 16, 128], BF16)
        h2p = [sbt(ph, "p7h%d" % i, [128, 16, 512], BF16) for i in range(4)]
        uGp = [sbt(ph, "p7u%d" % i, [128, 16, 512], BF16) for i in range(2)]
        gst = [sbt(ph, "p7g%d" % i, [128, 4, 512], BF16) for i in range(2)]
        ga_dv = ga_d.rearrange("c i t -> i c t")
        pre_banks = [psb[5], psb[6], psb[7]]

        def pre_units():
            nu = 0
            npb = 0
            for i in range(4):
                k.ld(h2p[i][0][:], h2T_d[:, :, i * 512:(i + 1) * 512], h2p[i][1], r=[h2T_b], w=[h2p[i][1]])
            for G in range(32):
                c0 = G * 4
                u_t, u_b = uGp[G % 2]
                k.ldc(u_t[:].rearrange("p k (c i) -> p k c i", c=4),
                      uT[:, c0:c0 + 4, :].rearrange("(k p) c i -> p k c i", p=128), u_b, w=[u_b])
                for tb in range(4):
                    g_t, g_b2 = gst[nu % 2]
                    nu += 1
                    for cc in range(4):
                        pa, pa_b = pre_banks[npb % 3]
                        npb += 1
                        for kk in range(16):
                            k.mm(pa[:], u_t[:, kk, cc * 128:(cc + 1) * 128], h2p[tb][0][:, kk, :], kk == 0, kk == 15,
                                 r=[u_b, h2p[tb][1]], w=[pa_b])
                        k.act(g_t[:, cc, :], pa[:], AF.Gelu_apprx_tanh, r=[pa_b], w=[g_b2])
                        if cc == 3:
                            k.ld(ga_dv[:, c0:c0 + 4, tb * 512:(tb + 1) * 512], g_t[:], g_b2, r=[g_b2], w=[ga_b])
                        yield
        pre_gen = pre_units()

        def pre_steps(n):
            for _ in range(n):
                next(pre_gen, None)
        s_sb, s_b = sbt(ph, "s_sb", [128, 16, 128], F32)
        scr, scr_b = sbt(ph, "scr", [128, 4, 128], F32)
        top, top_b = sbt(ph, "top", [128, 16, 16], F32)
        idxi, idxi_b = sbt(ph, "idxi", [128, 16, 16], I32)
        idxf, idxf_b = sbt(ph, "idxf", [128, 16, 16], F32)
        cand, cand_b = sbt(ph, "cand", [128, 8, 256], F32)
        cscr, cscr_b = sbt(ph, "cscr", [128, 4, 256], F32)
        ctop, ctop_b = sbt(ph, "ctop", [128, 8, 16], F32)
        sm, sm_b = sbt(ph, "sm", [128, 3, 8], F32)
        ee, ee_b = sbt(ph, "ee", [128, 8, 16], F32)
        ci, ci_b = sbt(ph, "ci", [128, 3, 8, 16], I32)
        cf, cf_b = sbt(ph, "cf", [128, 2, 8, 16], F32)
        abg, abg_b = sbt(ph, "abg", [128, 3, 8, 16], F32)
        abgT = [sbt(ph, "abgT%d" % i, [128, 3, 128], F32) for i in range(2)]
        NB_OH = 4
        At = [sbt(ph, "At%d" % i, [128, 8, 128], BF16) for i in range(NB_OH)]
        iob16, iob_b = sbt(ph, "iob16", [128, 128], BF16)
        tcopy(iob16[:], iof, r=[iot_b], w=[iob_b])
        aTb, aTb_b = sbt(ph, "aTb", [128, 128], BF16)
        gGt = [sbt(ph, "gGt%d" % i, [128, 8, 128], BF16) for i in range(NB_OH)]
        Wst = [sbt(ph, "Wst%d" % i, [128, 128, 128], BF16) for i in range(1)]
        W_dv = W_d.rearrange("c i t -> i c t")
        pwb = [psb[0], psb[1]]
        pwctr = [0]
        noh = 0
        def score_mm(hp, ts_):
            pss_, pss_b = psb[2 + (hp // 4) % 2]
            k.mm(pss_[:, (hp % 4) * 128:(hp % 4 + 1) * 128], qpT[:, hp, :], keys[:, hp, :], True, True,
                 r=[qpT_b, keys_b], w=[pss_b])

        def load_qp(t):
            k.ld(qpT[:], qpT_d[:, :, t * 128:(t + 1) * 128], qpT_b, r=[qpTd_b], w=[qpT_b])
        pre_issued = False
        for ob in range(4):
            for tl in range(4):
                t = ob * 4 + tl
                ts_ = slice(tl * 128, (tl + 1) * 128)
                if not pre_issued:
                    load_qp(t)
                for hp in range(16):
                    pss_, pss_b = psb[2 + (hp // 4) % 2]
                    if not (pre_issued and hp < 8):
                        score_mm(hp, ts_)
                    if hp % 4 == 3:
                        bk = hp // 4
                        dsti = s_sb[:, bk * 4:(bk + 1) * 4, :].rearrange("p a b -> p (a b)").bitcast(I32)
                        srci = pss_[:].bitcast(I32)
                        fw.op("dve", lambda dsti=dsti, srci=srci: nc.vector.tensor_single_scalar(
                            out=dsti, in_=srci, scalar=-128, op=ALU.bitwise_and), r=[pss_b], w=[s_b])
                s_i = s_sb[:].bitcast(I32)
                k.tt(s_i, s_i, io128.unsqueeze(1).to_broadcast([128, 16, 128]), ALU.bitwise_or, r=[s_b, iot_b], w=[s_b])
                pre_issued = False
                pre_steps(20)
                IL = 4
                for hp0 in range(0, 16, IL):
                    for hp in range(hp0, hp0 + IL):
                        fw.op("dve", lambda hp=hp: nc.vector.max(out=top[:, hp, 0:8], in_=s_sb[:, hp, :]), r=[s_b], w=[top_b])
                    for hp in range(hp0, hp0 + IL):
                        fw.op("dve", lambda hp=hp: nc.vector.match_replace(out=scr[:, hp % IL, :], in_to_replace=top[:, hp, 0:8],
                                                                          in_values=s_sb[:, hp, :], imm_value=-1e30),
                              r=[s_b, top_b], w=[scr_b])
                    for hp in range(hp0, hp0 + IL):
                        fw.op("dve", lambda hp=hp: nc.vector.max(out=top[:, hp, 8:16], in_=scr[:, hp % IL, :]), r=[scr_b], w=[top_b])
                fw.op("dve", lambda: nc.vector.tensor_single_scalar(out=idxi[:], in_=top[:].bitcast(I32), scalar=127,
                                                                   op=ALU.bitwise_and), r=[top_b], w=[idxi_b])
                tcopy(idxf[:], idxi[:], r=[idxi_b], w=[idxf_b])
                topv = top[:].rearrange("p (h two) k -> p h two k", two=2)
                idxv = idxf[:].rearrange("p (h two) k -> p h two k", two=2)
                cand4 = cand[:].rearrange("p h (i j) -> p h i j", i=16)
                k.tt(cand4, topv[:, :, 0, :].unsqueeze(3).to_broadcast([128, 8, 16, 16]),
                     topv[:, :, 1, :].unsqueeze(2).to_broadcast([128, 8, 16, 16]), ALU.add, r=[top_b], w=[cand_b])
                candi = cand[:].bitcast(I32)
                fw.op("dve", lambda candi=candi: nc.vector.tensor_single_scalar(out=candi, in_=candi, scalar=-256,
                                                                               op=ALU.bitwise_and), r=[cand_b], w=[cand_b])
                k.tt(candi, candi, io256.unsqueeze(1).to_broadcast([128, 8, 256]), ALU.bitwise_or, r=[cand_b, iot_b], w=[cand_b])
                for h0 in range(0, 8, IL):
                    for h in range(h0, h0 + IL):
                        fw.op("dve", lambda h=h: nc.vector.max(out=ctop[:, h, 0:8], in_=cand[:, h, :]), r=[cand_b], w=[ctop_b])
                    for h in range(h0, h0 + IL):
                        fw.op("dve", lambda h=h: nc.vector.match_replace(out=cscr[:, h % IL, :], in_to_replace=ctop[:, h, 0:8],
                                                                        in_values=cand[:, h, :], imm_value=-1e30),
                              r=[cand_b, ctop_b], w=[cscr_b])
                    for h in range(h0, h0 + IL):
                        fw.op("dve", lambda h=h: nc.vector.max(out=ctop[:, h, 8:16], in_=cscr[:, h % IL, :]), r=[cscr_b], w=[ctop_b])
                k.ts(sm[:, 0, :], ctop[:, :, 0], -1.0, None, ALU.mult, None, r=[ctop_b], w=[sm_b])
                for h in range(8):
                    k.act(ee[:, h, :], ctop[:, h, :], AF.Exp, r=[ctop_b, sm_b], w=[ee_b, sm_b], bias=sm[:, 0, h:h + 1],
                          accum_out=sm[:, 1, h:h + 1])
                k.recip(sm[:, 2, :], sm[:, 1, :], r=[sm_b], w=[sm_b])
                k.tt(abg[:, 2, :, :], ee[:], sm[:, 2, :].unsqueeze(2).to_broadcast([128, 8, 16]), ALU.mult,
                     r=[ee_b, sm_b], w=[abg_b])
                fw.op("dve", lambda: nc.vector.tensor_single_scalar(out=ci[:, 0], in_=ctop[:].bitcast(I32), scalar=255,
                                                                   op=ALU.bitwise_and), r=[ctop_b], w=[ci_b])
                fw.op("dve", lambda: nc.vector.tensor_single_scalar(out=ci[:, 1], in_=ci[:, 0], scalar=4,
                                                                   op=ALU.arith_shift_right), r=[ci_b], w=[ci_b])
                fw.op("dve", lambda: nc.vector.tensor_single_scalar(out=ci[:, 2], in_=ci[:, 0], scalar=15,
                                                                   op=ALU.bitwise_and), r=[ci_b], w=[ci_b])
                tcopy(cf[:], ci[:, 1:3], r=[ci_b], w=[cf_b])
                for w_ in range(2):
                    k.tt(cand[:].rearrange("p h (i j) -> p h i j", i=16), cf[:, w_].unsqueeze(3).to_broadcast([128, 8, 16, 16]),
                         iot[:, 0:16].unsqueeze(1).unsqueeze(1).to_broadcast([128, 8, 16, 16]), ALU.is_equal,
                         r=[cf_b, iot_b], w=[cand_b])
                    k.tt(cand[:].rearrange("p h (i j) -> p h i j", i=16), cand[:].rearrange("p h (i j) -> p h i j", i=16), idxv[:, :, w_, :].unsqueeze(2).to_broadcast([128, 8, 16, 16]), ALU.mult,
                         r=[cand_b, idxf_b], w=[cand_b])
                    k.red(abg[:, w_], cand[:].rearrange("p h (i j) -> p h i j", i=16), ALU.add, r=[cand_b], w=[abg_b])
                ptr, ptr_b = psb[4]
                for w_ in range(3):
                    k.tr(ptr[:, w_ * 128:(w_ + 1) * 128], abg[:, w_].rearrange("p h k -> p (h k)"), cmat[:, 0, :],
                         r=[abg_b, cmat_b], w=[ptr_b])
                aT_t, aT_b = abgT[t % 2]
                k.act(aT_t[:].rearrange("p a b -> p (a b)"), ptr[:, 0:384], AF.Copy, r=[ptr_b], w=[aT_b])
                ws_t, ws_b = Wst[0]
                TG = 8
                tcopy(aTb[:], aT_t[:, 0, :], r=[aT_b], w=[aTb_b])
                pend7 = []
                LAG = 2
                for g8 in range(128 // TG):
                    tk0 = g8 * TG
                    A_t, A_b = At[noh % NB_OH]
                    G_t, G_b = gGt[noh % NB_OH]
                    noh += 1
                    k.tt(A_t[:], iob16[:].unsqueeze(1).to_broadcast([128, TG, 128]),
                         aTb[:, tk0:tk0 + TG].unsqueeze(2).to_broadcast([128, TG, 128]), ALU.is_equal,
                         r=[iob_b, aTb_b], w=[A_b])
                    for qq in range(TG):
                        k.ts(G_t[:, qq, :], iob16[:], aT_t[:, 1, tk0 + qq:tk0 + qq + 1], aT_t[:, 2, tk0 + qq:tk0 + qq + 1],
                             ALU.is_equal, ALU.mult, r=[iob_b, aT_b], w=[G_b])

                    def wmm(A_t=A_t, A_b=A_b, G_t=G_t, G_b=G_b, tk0=tk0):
                        nonlocal_npw = pwctr
                        for q4 in range(TG // 4):
                            pw, pw_b = pwb[nonlocal_npw[0] % 2]
                            nonlocal_npw[0] += 1
                            for qq in range(4):
                                k.mm(pw[:, qq * 128:(qq + 1) * 128], G_t[:, q4 * 4 + qq, :], A_t[:, q4 * 4 + qq, :], True, True,
                                     r=[A_b, G_b], w=[pw_b])
                            k.act(ws_t[:, :, tk0 + q4 * 4:tk0 + (q4 + 1) * 4], pw[:].rearrange("p (t c) -> p c t", t=4), AF.Copy,
                                  r=[pw_b], w=[ws_b])
                    pend7.append(wmm)
                    if len(pend7) > LAG:
                        pend7.pop(0)()
                    if g8 % 4 != 3:
                        pre_steps(1)
                    if g8 == 11 and t < 15:
                        load_qp(t + 1)
                        for hp in range(8):
                            score_mm(hp, None)
                        pre_issued = True
                while pend7:
                    pend7.pop(0)()
                k.ld(W_dv[:, :, t * 128:(t + 1) * 128], ws_t[:], ws_b, r=[ws_b], w=[W_b])
        for _ in pre_gen:
            pass
        if debug:
            da, da_b = k.dscr("dbg_abgT", [128, 3, 128], F32)
            k.ld(da, abgT[1][0][:], abgT[1][1], r=[abgT[1][1]], w=[da_b])
            dbgb.append(da_b)
        end_phase()
    if stop_after <= 7:
        return finish()

    with ExitStack() as ph:
        acc_bufs = [Buf("acc%d" % i) for i in range(4)]
        grow, grow_b = load_grow(ph, "grow8")
        acc, acc_b = sbt(ph, "acc", [128, 4, D], F32)
        GC = 8
        uG = [sbt(ph, "uG%d" % i, [128, GC, 512], BF16) for i in range(2)]
        vG = [sbt(ph, "vG%d" % i, [128, GC, D], BF16) for i in range(2)]
        WG = [sbt(ph, "WG%d" % i, [128, GC, 512], BF16) for i in range(2)]
        actT = [sbt(ph, "actT%d" % i, [128, GC, 512], BF16) for i in range(2)]
        x1t = [sbt(ph, "p8x%d" % i, [128, D], F32) for i in range(2)]
        fst = [sbt(ph, "fst%d" % i, [128, 2], F32) for i in range(2)]
        junk, junk_b = sbt(ph, "p8junk", [128, D], BF16)
        gi = 0
        for tb in range(4):
            tbs = slice(tb * 512, (tb + 1) * 512)
            for G in range(128 // GC):
                c0 = G * GC
                u_t, u_b = uG[gi % 2]
                v_t, v_b2 = vG[gi % 2]
                w_t, w_b = WG[gi % 2]
                a_t, a_b = actT[gi % 2]
                gi += 1
                for hv in range(GC // 4):
                    k.ldc(v_t[:, hv * 4:(hv + 1) * 4, :], vP[c0 + hv * 4:c0 + (hv + 1) * 4].rearrange("c i d -> i c d"), v_b2, w=[v_b2])
                k.ld(w_t[:], W_dv[:, c0:c0 + GC, tbs], w_b, r=[W_b], w=[w_b])
                k.ld(u_t[:], ga_d.rearrange("c i t -> i c t")[:, c0:c0 + GC, tbs], u_b, r=[ga_b], w=[u_b])
                for hv in range(GC // 4):
                    k.tt(a_t[:, hv * 4:(hv + 1) * 4, :], u_t[:, hv * 4:(hv + 1) * 4, :], w_t[:, hv * 4:(hv + 1) * 4, :], ALU.mult,
                         r=[u_b, w_b], w=[a_b])
                for tl in range(4):
                    ts_ = slice(tl * 128, (tl + 1) * 128)
                    for nb in range(4):
                        ns = slice(nb * 512, (nb + 1) * 512)
                        po, po_b = psb[(4 * (tl % 2) + nb)]
                        for cc in range(GC):
                            k.mm(po[:], a_t[:, cc, ts_], v_t[:, cc, ns], cc == 0, cc == GC - 1, r=[a_b, v_b2], w=[po_b])
                        if G == 0:
                            k.act(acc[:, tl, ns], po[:], AF.Copy, r=[po_b], w=[acc_bufs[tl]])
                        else:
                            k.tt(acc[:, tl, ns], acc[:, tl, ns], po[:], ALU.add, r=[acc_bufs[tl], po_b], w=[acc_bufs[tl]])
            for tl in range(4):
                t = tb * 4 + tl
                x_t, x_b = x1t[t % 2]
                s_t, s_b2 = fst[t % 2]
                k.ld(x_t[:], x1_d[t * 128:(t + 1) * 128, :], x_b, r=[x1_b], w=[x_b])
                k.tt(acc[:, tl, :], acc[:, tl, :], grow[:, 1, :], ALU.mult, r=[acc_bufs[tl], grow_b], w=[acc_bufs[tl]])
                k.tt(x_t[:], x_t[:], acc[:, tl, :], ALU.add, r=[x_b, acc_bufs[tl]], w=[x_b])
                k.act(junk[:], x_t[:], AF.Square, r=[x_b], w=[junk_b, s_b2], accum_out=s_t[:, 0:1])
                k.act(s_t[:, 1:2], s_t[:, 0:1], AF.Sqrt, r=[s_b2], w=[s_b2], scale=1.0 / D, bias=EPS)
                k.recip(s_t[:, 1:2], s_t[:, 1:2], r=[s_b2], w=[s_b2])
                k.ts(x_t[:], x_t[:], s_t[:, 1:2], None, ALU.mult, None, r=[x_b, s_b2], w=[x_b])
                k.tt(x_t[:], x_t[:], grow[:, 2, :], ALU.mult, r=[x_b, grow_b], w=[x_b])
                k.ld(out_d[t * 128:(t + 1) * 128, :], x_t[:], x_b, r=[x_b], w=[out_b])
        end_phase()
    return finish()


def _rope_tables(pos_list):
    pos = np.asarray(pos_list)
    n_freq = 32
    inv = (np.float32(10000.0) ** (-np.arange(n_freq, dtype=np.float32) / np.float32(n_freq))).astype(np.float32)
    p = np.maximum(pos, 0)
    row = (p // 64).astype(np.float32)
    col = (p % 64).astype(np.float32)
    ang = np.concatenate([row[:, None] * inv[None, :], col[:, None] * inv[None, :]], axis=1).astype(np.float32)
    cos = np.cos(ang).astype(np.float32)
    sin = np.sin(ang).astype(np.float32)
    isctx = pos < 0
    cos[isctx] = 1.0
    sin[isctx] = 0.0
    cosT = np.concatenate([cos, cos], axis=1).T
    sinS = np.concatenate([-sin, sin], axis=1).T
    return np.ascontiguousarray(cosT, dtype=np.float32), np.ascontiguousarray(sinS, dtype=np.float32)


def _consts():
    p = np.arange(128)
    ident = np.eye(128, dtype=np.float32)
    pswap = np.zeros((128, 128), np.float32)
    pswap[(p + 64) % 128, p] = 1.0
    kk, qq = np.meshgrid(p, p, indexing="ij")
    D1 = np.maximum(qq - kk, 0).astype(np.float32)
    U1 = (qq >= kk).astype(np.float32)
    D2 = np.maximum(kk - qq, 0).astype(np.float32)
    U2 = (kk >= qq).astype(np.float32)
    POSF = np.tile((p + 1).astype(np.float32)[None, :], (128, 1))
    POSB = np.tile((128 - p).astype(np.float32)[None, :], (128, 1))
    cmat = np.stack([ident, pswap, D1, U1, D2, U2, POSF, POSB], axis=1)
    kpos = np.concatenate([np.tile((127 - p).astype(np.float32)[:, None], (1, 8)),
                           np.tile(p.astype(np.float32)[:, None], (1, 8))], axis=1)
    n = (np.arange(16)[None, :] * 128 + p[:, None]).astype(np.float32)
    npos = np.stack([-(n - 1024.0), n - 1023.0], axis=1).astype(np.float32)
    nn = np.arange(2048, dtype=np.float32)
    qpos = np.stack([np.tile((nn - 1024.0)[None, :], (128, 1)), np.tile((1023.0 - nn)[None, :], (128, 1))], axis=1).astype(np.float32)
    iot = np.zeros((128, 512), np.float32)
    iot[:, 0:128] = np.arange(128, dtype=np.float32)[None, :]
    iot[:, 128:384] = np.arange(256, dtype=np.int32).view(np.float32)[None, :]
    iot[:, 384:512] = np.arange(128, dtype=np.int32).view(np.float32)[None, :]
    return np.ascontiguousarray(cmat), np.ascontiguousarray(kpos), iot, np.ascontiguousarray(npos), np.ascontiguousarray(qpos)


def _dist(half):
    BIG = np.float32(1e6)
    d = np.zeros((128, 2, NT_ALL), np.float32)
    p = np.arange(128, dtype=np.float32)
    for t in range(2):
        c = t * 128 + p
        d[:, 0, t] = (255 - c) + (2048 if half == 1 else 0) + 1025
        d[:, 1, t] = c + (2048 if half == 0 else 0) + 1025
    for t in range(2, 18):
        loc = (t - 2) * 128 + p
        if half == 1:
            d[:, 0, t] = 2047 - loc + 1025
            d[:, 1, t] = BIG
        else:
            d[:, 0, t] = BIG
            d[:, 1, t] = loc + 1025
    return d


def prep(inputs, cores=range(8)):
    f = lambda a: np.ascontiguousarray(np.asarray(a), dtype=np.float32)
    x = f(inputs["x"]); c = f(inputs["c"]); ctx = f(inputs["ctx"]); c_ctx = f(inputs["c_ctx"])
    cmat, kpos, iot, npos, qpos = _consts()
    shared = {
        "ada_w": f(inputs["ada_w"])[0],
        "ada_bT": np.ascontiguousarray(f(inputs["ada_b"])[0].reshape(96, 128).T),
        "n1w": np.ascontiguousarray(f(inputs["norm1_w"])[0].reshape(16, 128).T),
        "n2w": np.ascontiguousarray(f(inputs["norm2_w"])[0].reshape(16, 128).T),
        "nfw": f(inputs["norm_f_w"]),
        "w_in": f(inputs["w_in"])[0],
        "qkw": np.ascontiguousarray(np.stack([f(inputs["q_norm_w"])[0], f(inputs["k_norm_w"])[0]], axis=1)),
        "zdec": np.concatenate([f(inputs["ret_decay_fwd"])[0], f(inputs["ret_decay_bwd"])[0]]),
        "rnw": f(inputs["ret_norm_w"])[0],
        "w_ab": f(inputs["w_attn_branch"])[0],
        "w_rb": f(inputs["w_ret_branch"])[0],
        "w_mo": f(inputs["w_merge_out"])[0],
        "pwq": f(inputs["peer_w_q"])[0],
        "keysT": np.ascontiguousarray(f(inputs["peer_keys"])[0].reshape(16, 128, 128).transpose(2, 0, 1)),
        "uT": np.ascontiguousarray(f(inputs["peer_u"])[0].reshape(128, 128, D).transpose(2, 0, 1)),
        "vP": f(inputs["peer_v"])[0].reshape(128, 128, D),
        "cmat": cmat, "kpos": kpos, "iotas": iot, "npos": npos, "qpos": qpos,
    }
    maps = []
    for core in cores:
        b, half = core // 2, core % 2
        own = np.arange(half * 2048, (half + 1) * 2048)
        oth = np.arange((1 - half) * 2048, (2 - half) * 2048)
        pos = np.concatenate([-np.ones(256, np.int64), oth, own])
        cosT, sinS = _rope_tables(pos)
        m = dict(shared)
        m["tok"] = np.ascontiguousarray(np.concatenate([ctx[b], x[b][oth], x[b][own]], axis=0))
        m["cT"] = np.ascontiguousarray(np.stack([c[b].reshape(16, 128).T, c_ctx.reshape(16, 128).T], axis=2))
        m["cosT"] = cosT
        m["sinS"] = sinS
        m["dist"] = _dist(half)
        maps.append(m)
    return maps


_NC_CACHE = {}


def kernel(**inputs):
    if "nc" not in _NC_CACHE:
        _NC_CACHE["nc"] = build()
    nc = _NC_CACHE["nc"]
    maps = prep(inputs)
    res = run_bass_kernel_spmd(nc, maps, core_ids=list(range(8)))
    out = np.zeros((4, SEQ, D), np.float32)
    for core in range(8):
        b, half = core // 2, core % 2
        out[b, half * 2048:(half + 1) * 2048, :] = np.asarray(res.results[core]["out"], dtype=np.float32)
    return out
```

```python
import numpy as np
from contextlib import ExitStack
import concourse.bass as bass
import concourse.mybir as mybir
from concourse.bass_utils import run_bass_kernel_spmd

F32 = mybir.dt.float32
BF16 = mybir.dt.bfloat16
I32 = mybir.dt.int32
AF = mybir.ActivationFunctionType
ALU = mybir.AluOpType
AX = mybir.AxisListType

D = 2048
SEQ = 4096
CTX = 256
NT_ALL = 34
NTOK = NT_ALL * 128
OWN0 = 18
NOWN = 16
INW = 11776
EPS = 1e-6
C_QA, C_KA, C_VA, C_QR, C_KR, C_VR, C_GR, C_GA, C_GB = 0, 1024, 1280, 1536, 2560, 3584, 5632, 7680, 9728
SEM_LIMIT = 12000


class Buf:
    def __init__(self, name, accum=False):
        self.name = name
        self.w = {}
        self.r = {}
        self.accum = accum
        self.sem = None
        self.semcnt = 0


class Eng:
    def __init__(self, fw, name, handle):
        self.fw, self.name, self.h = fw, name, handle
        self.n = 0
        self.sems = []
        self.waited = {}
        self.thunks = []

    def cur_token(self):
        e = (self.n - 1) // SEM_LIMIT
        return ((self.name, e), (self.n - 1) % SEM_LIMIT + 1)


class FW:
    def __init__(self, nc, stack):
        self.nc, self.stack = nc, stack
        self.semmap = {}
        self.nsem = 0
        self.dma_owners = []
        self.E = {
            "pe": Eng(self, "pe", nc.tensor), "act": Eng(self, "act", nc.scalar),
            "dve": Eng(self, "dve", nc.vector), "pool": Eng(self, "pool", nc.gpsimd),
            "sp": Eng(self, "sp", nc.sync),
        }

    def newsem(self, key):
        s = self.stack.enter_context(self.nc.semaphore("s%d" % self.nsem))
        self.nsem += 1
        self.semmap[key] = s
        return s

    def _need(self, eng, toks, skip_same=False):
        for key, val in toks.items():
            if key[0] == "pe" and eng.name == "pe":
                continue
            if skip_same and key[0] == eng.name:
                continue
            if eng.waited.get(key, 0) >= val:
                continue
            if key[0] in self.E:
                later = [k for k in eng.waited if k[0] == key[0] and k[1] > key[1]]
                if later:
                    continue
            eng.waited[key] = val
            sem = self.semmap[key]
            eng.thunks.append((lambda h=eng.h, sem=sem, val=val: h.wait_ge(sem, val)))

    def op(self, en, fn, r=(), w=()):
        eng = self.E[en]
        for b in r:
            self._need(eng, b.w)
        for b in w:
            if not b.accum:
                self._need(eng, b.w, skip_same=True)
                self._need(eng, b.r, skip_same=True)
        eng.n += 1
        key, val = eng.cur_token()
        if key not in self.semmap:
            self.newsem(key)
        sem = self.semmap[key]
        eng.thunks.append((lambda fn=fn, sem=sem: fn().then_inc(sem, 1)))
        for b in r:
            b.r[key] = max(b.r.get(key, 0), val)
        for b in w:
            if b.accum:
                b.w[key] = max(b.w.get(key, 0), val)
            else:
                b.w = {key: val}
                b.r = {}

    def dma(self, en, out_ap, in_ap, owner, r=(), w=()):
        eng = self.E[en]
        for b in r:
            self._need(eng, b.w)
        for b in w:
            if not b.accum:
                self._need(eng, b.w)
                self._need(eng, b.r)
        if owner.sem is None:
            owner.sem = ("dma", owner.name)
            self.newsem(owner.sem)
            self.dma_owners.append(owner)
        owner.semcnt += 16
        key, val = owner.sem, owner.semcnt
        sem = self.semmap[key]
        h = eng.h
        eng.thunks.append((lambda h=h, o=out_ap, i=in_ap, sem=sem: h.dma_start(out=o, in_=i).then_inc(sem, 16)))
        for b in r:
            b.r[key] = max(b.r.get(key, 0), val)
        for b in w:
            if b.accum:
                b.w[key] = max(b.w.get(key, 0), val)
            else:
                b.w = {key: val}
                b.r = {}

    def drain(self, en, bufs):
        eng = self.E[en]
        for b in bufs:
            self._need(eng, b.w)
            self._need(eng, b.r)

    def barrier(self):
        toks = {}
        for e in self.E.values():
            if e.n > 0:
                key, val = e.cur_token()
                toks[key] = val
        for o in self.dma_owners:
            toks[o.sem] = o.semcnt
        for e in self.E.values():
            self._need(e, dict(toks))

    def emit(self):
        nc = self.nc
        with nc.Block() as block:
            @block.tensor
            def _(t):
                for th in self.E["pe"].thunks:
                    th()

            @block.scalar
            def _(t):
                for th in self.E["act"].thunks:
                    th()

            @block.vector
            def _(t):
                for th in self.E["dve"].thunks:
                    th()

            @block.gpsimd
            def _(t):
                for th in self.E["pool"].thunks:
                    th()

            @block.sync
            def _(t):
                for th in self.E["sp"].thunks:
                    th()
        for e in self.E.values():
            e.thunks = []


class K:
    def __init__(self, debug=False, stop_after=99):
        self.debug = debug
        self.stop_after = stop_after
        self.stack = ExitStack()
        self.nc = bass.Bass("TRN2", target_bir_lowering=False)
        self.fw = FW(self.nc, self.stack)
        self.nbuf = 0

    def din(self, name, shape, dt=F32):
        return self.nc.dram_tensor(name, list(shape), dt, kind="ExternalInput").ap()

    def dscr(self, name, shape, dt, out=False):
        kind = "ExternalOutput" if (out or (self.debug and (self.debug is True or name in self.debug))) else "Internal"
        ap = self.nc.dram_tensor(name, list(shape), dt, kind=kind).ap()
        return ap, Buf(name, accum=True)

    def sb(self, name, shape, dt):
        t = self.stack.enter_context(self.nc.sbuf_tensor("sb_" + name, list(shape), dt))
        return t, Buf(name)

    def ps(self, name, shape, dt):
        t = self.stack.enter_context(self.nc.psum_tensor(name, list(shape), dt))
        return t, Buf(name)

    def mm(self, out, lhsT, rhs, start, stop, r, w):
        nc = self.nc
        self.fw.op("pe", lambda: nc.tensor.matmul(out, lhsT, rhs, start=start, stop=stop), r=r, w=w)

    def tr(self, out, in_, ident, r, w):
        nc = self.nc
        self.fw.op("pe", lambda: nc.tensor.transpose(out, in_, ident), r=r, w=w)

    def act(self, out, in_, func, r, w, bias=None, scale=None, accum_out=None):
        nc = self.nc
        kw = {}
        if bias is not None:
            kw["bias"] = bias
        if scale is not None:
            kw["scale"] = scale
        if accum_out is not None:
            kw["accum_out"] = accum_out
        self.fw.op("act", lambda: nc.scalar.activation(out=out, in_=in_, func=func, **kw), r=r, w=w)

    def tt(self, out, in0, in1, op, r, w, eng="dve"):
        h = self.nc.vector if eng == "dve" else self.nc.gpsimd
        self.fw.op(eng, lambda: h.tensor_tensor(out=out, in0=in0, in1=in1, op=op), r=r, w=w)

    def ts(self, out, in0, s1, s2, op0, op1, r, w, eng="dve"):
        h = self.nc.vector if eng == "dve" else self.nc.gpsimd
        if s2 is None:
            self.fw.op(eng, lambda: h.tensor_scalar(out=out, in0=in0, scalar1=s1, scalar2=None, op0=op0), r=r, w=w)
        else:
            self.fw.op(eng, lambda: h.tensor_scalar(out=out, in0=in0, scalar1=s1, scalar2=s2, op0=op0, op1=op1), r=r, w=w)

    def stt(self, out, in0, scalar, in1, op0, op1, r, w):
        nc = self.nc
        self.fw.op("dve", lambda: nc.vector.scalar_tensor_tensor(out=out, in0=in0, scalar=scalar, in1=in1, op0=op0, op1=op1), r=r, w=w)

    def red(self, out, in_, op, r, w):
        nc = self.nc
        self.fw.op("dve", lambda: nc.vector.tensor_reduce(out=out, in_=in_, axis=AX.X, op=op), r=r, w=w)

    def recip(self, out, in_, r, w):
        nc = self.nc
        self.fw.op("dve", lambda: nc.vector.reciprocal(out=out, in_=in_), r=r, w=w)

    def ld(self, out_ap, in_ap, owner, r=(), w=(), q="sp"):
        self.fw.dma(q, out_ap, in_ap, owner, r=r, w=w)

    def ldc(self, out_ap, in_ap, owner, r=(), w=()):
        self.fw.dma("pool", out_ap, in_ap, owner, r=r, w=w)


def build(debug=False, stop_after=99):
    k = K(debug=debug, stop_after=stop_after)
    nc, fw = k.nc, k.fw
    tok = k.din("tok", [NTOK, D])
    cT_in = k.din("cT", [128, 16, 2])
    ada_w = k.din("ada_w", [D, 6 * D])
    ada_bT = k.din("ada_bT", [128, 96])
    n1w_in = k.din("n1w", [128, 16])
    n2w_in = k.din("n2w", [128, 16])
    nfw_in = k.din("nfw", [D])
    w_in = k.din("w_in", [D, INW])
    qkw_in = k.din("qkw", [128, 2])
    zdec_in = k.din("zdec", [16])
    rnw_in = k.din("rnw", [D])
    w_ab = k.din("w_ab", [1024, D])
    w_rb = k.din("w_rb", [D, D])
    w_mo = k.din("w_mo", [D, D])
    pwq = k.din("pwq", [D, D])
    keysT_in = k.din("keysT", [128, 16, 128])
    uT = k.din("uT", [D, 128, 128])
    vP = k.din("vP", [128, 128, D])
    cosT_in = k.din("cosT", [128, NTOK])
    sinS_in = k.din("sinS", [128, NTOK])
    cmat_in = k.din("cmat", [128, 8, 128])
    kpos_in = k.din("kpos", [128, 16])
    dist_in = k.din("dist", [128, 2, NT_ALL])
    npos_in = k.din("npos", [128, 2, 16])
    qpos_in = k.din("qpos", [128, 2, NOWN * 128])
    iota_in = k.din("iotas", [128, 512])
    out_d = nc.dram_tensor("out", [NOWN * 128, D], F32, kind="ExternalOutput").ap()
    out_b = Buf("out", accum=True)

    hT_d, hT_b = k.dscr("hT_d", [128, 16, NTOK], BF16)
    gsc_d, gsc_b = k.dscr("gsc_d", [2, D], F32)
    attnT_d, attnT_b = k.dscr("attnT_d", [128, 8, NOWN * 128], BF16)
    retT_d, retT_b = k.dscr("retT_d", [128, 16, NOWN * 128], BF16)
    mixT_d, mixT_b = k.dscr("mixT_d", [128, 16, NOWN * 128], BF16)
    x1_d, x1_b = k.dscr("x1_d", [NOWN * 128, D], F32)
    h2T_d, h2T_b = k.dscr("h2T_d", [128, 16, NOWN * 128], BF16)
    W_d, W_b = k.dscr("W_d", [128, 128, NOWN * 128], BF16)
    ga_d, ga_b = k.dscr("ga_d", [128, 128, NOWN * 128], BF16)
    qpT_d, qpTd_b = k.dscr("qpT_d", [128, 16, NOWN * 128], BF16)
    dbgb = []

    cmat, cmat_b = k.sb("cmat", [128, 8, 128], F32)
    k.ld(cmat[:], cmat_in, cmat_b, w=[cmat_b])
    cbf, cbf_b = k.sb("cbf", [128, 3, 128], BF16)
    k.ldc(cbf[:, 0:2, :], cmat_in[:, 0:2, :], cbf_b, w=[cbf_b])
    fw.op("dve", lambda: nc.vector.memset(cbf[:, 2, :], 1.0), w=[cbf_b])
    ident = cbf[:, 0, :]
    pswap = cbf[:, 1, :]
    ones = cbf[:, 2, :]
    vecs, vecs_b = k.sb("vecs", [128, 10, 16], F32)
    k.ld(vecs[:, 0, :], n1w_in, vecs_b, w=[vecs_b])
    k.ld(vecs[:, 1, :], n2w_in, vecs_b, w=[vecs_b])
    N1W, N2W, G1, G1C, SH1, SH1C, G2, SH2, TMPA, TMPB = range(10)
    mods, mods_b = k.sb("mods", [128, 96, 2], F32)
    psb = [k.ps("ps%d" % i, [128, 512], F32) for i in range(8)]

    def sbt(ph, name, shape, dt):
        return ph.enter_context(nc.sbuf_tensor("sb_" + name, list(shape), dt)), Buf(name)

    def end_phase():
        fw.barrier()
        fw.emit()

    def finish():
        allb = [out_b, hT_b, gsc_b, attnT_b, retT_b, mixT_b, x1_b, h2T_b, W_b, ga_b, qpTd_b] + dbgb
        fw.drain("sp", allb)
        fw.barrier()
        fw.emit()
        return nc

    def memset(ap, val, w, eng="dve"):
        h = nc.vector if eng == "dve" else nc.gpsimd
        fw.op(eng, lambda: h.memset(ap, val), w=w)

    def tcopy(out, in_, r, w, eng="dve"):
        h = nc.vector if eng == "dve" else nc.gpsimd
        fw.op(eng, lambda: h.tensor_copy(out=out, in_=in_), r=r, w=w)

    cT, cT_b = k.sb("cTs", [128, 16, 2], F32)
    k.ld(cT[:], cT_in, cT_b, w=[cT_b])
    sc, sc_b = k.sb("sc", [128, 16, 2], BF16)
    k.act(sc[:], cT[:], AF.Silu, r=[cT_b], w=[sc_b])
    abT, abT_b = k.sb("abT", [128, 96], F32)
    k.ld(abT[:], ada_bT, abT_b, w=[abT_b])
    pm, pm_b = psb[7]

    ada_stack = ExitStack()
    wblk = [sbt(ada_stack, "adaw%d" % i, [128, 16, 1024], BF16) for i in range(2)]

    def ada_load(blk, buf=None):
        if blk >= 12:
            return
        wt, wb = buf if buf is not None else wblk[blk % 2]
        k.ldc(wt[:], ada_w[:, blk * 1024:(blk + 1) * 1024].rearrange("(c p) n -> p c n", p=128), wb, w=[wb])

    def ada_mm(blk, buf=None):
        wt, wb = buf if buf is not None else wblk[blk % 2]
        for j in range(8):
            col = (blk * 8 + j) * 2
            for c in range(16):
                k.mm(pm[:, col:col + 2], wt[:, c, j * 128:(j + 1) * 128], sc[:, c, :], c == 0, c == 15,
                     r=[wb, sc_b], w=[pm_b])

    def ada_mods(c0, c1):
        k.tt(mods[:, c0:c1, :], pm[:, 2 * c0:2 * c1].rearrange("p (a s) -> p a s", s=2),
             abT[:, c0:c1].unsqueeze(2).to_broadcast([128, c1 - c0, 2]), ALU.add, r=[pm_b, abT_b], w=[mods_b])

    def plus1(dst, src):
        k.ts(vecs[:, dst, :], src, 1.0, None, ALU.add, None, r=[mods_b], w=[vecs_b])

    def cpv(dst, src):
        k.ts(vecs[:, dst, :], src, 0.0, None, ALU.add, None, r=[mods_b], w=[vecs_b])

    def ada_finish_B():
        ada_mods(32, 96)
        plus1(TMPA, mods[:, 64:80, 0])
        k.tt(vecs[:, G2, :], vecs[:, TMPA, :], vecs[:, N2W, :], ALU.mult, r=[vecs_b], w=[vecs_b])
        cpv(SH2, mods[:, 48:64, 0])
        cpv(TMPA, mods[:, 32:48, 0])
        cpv(TMPB, mods[:, 80:96, 0])
        gst_b = Buf("gst")
        for gi, slot in ((0, TMPA), (1, TMPB)):
            dst = gsc_d[gi].rearrange("(c p) -> p c", p=128)
            src = vecs[:, slot, :]
            eng = fw.E["sp"]
            fw._need(eng, vecs_b.w)
            if gst_b.sem is None:
                gst_b.sem = ("dma", "gst")
                fw.newsem(gst_b.sem)
                fw.dma_owners.append(gst_b)
            gst_b.semcnt += 16
            sem = fw.semmap[gst_b.sem]
            eng.thunks.append(lambda dst=dst, src=src, sem=sem: nc.sync.dma_start(
                out=dst, in_=src, allow_slow_non_contiguous=True).then_inc(sem, 16))
            gsc_b.w[gst_b.sem] = gst_b.semcnt
            vecs_b.r[gst_b.sem] = gst_b.semcnt

    with ExitStack() as ph:
        ada_load(0)
        for blk in range(4):
            if blk < 3:
                ada_load(blk + 1)
            ada_mm(blk)
        ada_mods(0, 32)
        plus1(TMPA, mods[:, 16:32, 0])
        k.tt(vecs[:, G1, :], vecs[:, TMPA, :], vecs[:, N1W, :], ALU.mult, r=[vecs_b], w=[vecs_b])
        plus1(TMPA, mods[:, 16:32, 1])
        k.tt(vecs[:, G1C, :], vecs[:, TMPA, :], vecs[:, N1W, :], ALU.mult, r=[vecs_b], w=[vecs_b])
        cpv(SH1, mods[:, 0:16, 0])
        cpv(SH1C, mods[:, 0:16, 1])
        end_phase()
    ada_stack.close()

    def norm_to_T(ph_name, ntiles, load_tile, gsel, blkinfo, store_blk, hook=None):
        with ExitStack() as ph:
            xs = [sbt(ph, ph_name + "xs%d" % i, [128, D], BF16) for i in range(2)]
            junk, junk_b = sbt(ph, ph_name + "junk", [128, D], BF16)
            st = [sbt(ph, ph_name + "st%d" % i, [128, 2], F32) for i in range(2)]
            hblk = [sbt(ph, ph_name + "hblk%d" % i, [128, 16, 512], BF16) for i in range(2)]
            hblk = [(t_, (b_, Buf(b_.name + "D"))) for (t_, b_) in hblk]
            ctx_ = {"ph": ph}
            def stats(t):
                x_t, x_b = load_tile(ctx_, t)
                xs_t, xs_b = xs[t % 2]
                s_t, s_b = st[t % 2]
                k.act(junk[:], x_t[:], AF.Square, r=[x_b, s_b], w=[junk_b, s_b], accum_out=s_t[:, 0:1])
                k.act(s_t[:, 1:2], s_t[:, 0:1], AF.Sqrt, r=[s_b], w=[s_b], scale=1.0 / D, bias=EPS)
                k.recip(s_t[:, 1:2], s_t[:, 1:2], r=[s_b], w=[s_b])
                k.ts(xs_t[:], x_t[:], s_t[:, 1:2], None, ALU.mult, None, r=[x_b, s_b], w=[xs_b])
            stats(0)
            for t in range(ntiles):
                xs_t, xs_b = xs[t % 2]
                blk, pos, bw, b0, last = blkinfo(t)
                h_t, h_b = hblk[blk % 2]
                if t + 1 < ntiles:
                    stats(t + 1)
                pa, pa_b = psb[2 * (t % 2)]
                pb, pb_b = psb[2 * (t % 2) + 1]
                gv, sv = gsel(t)
                for c in range(16):
                    pt_, ptb = (pa, pa_b) if c < 8 else (pb, pb_b)
                    pv = pt_[:].bitcast(BF16)
                    cc = c % 8
                    k.tr(pv[:, cc * 128:(cc + 1) * 128], xs_t[:, c * 128:(c + 1) * 128], ident, r=[xs_b, cbf_b], w=[ptb])
                for c in range(16):
                    pt_, ptb = (pa, pa_b) if c < 8 else (pb, pb_b)
                    pv = pt_[:].bitcast(BF16)
                    cc = c % 8
                    if c < 8:
                        k.act(h_t[:, c, pos * 128:(pos + 1) * 128], pv[:, cc * 128:(cc + 1) * 128], AF.Identity,
                              r=[ptb, vecs_b], w=[h_b[0]], scale=vecs[:, gv, c:c + 1], bias=vecs[:, sv, c:c + 1])
                    else:
                        k.ts(h_t[:, c, pos * 128:(pos + 1) * 128], pv[:, cc * 128:(cc + 1) * 128], vecs[:, gv, c:c + 1],
                             vecs[:, sv, c:c + 1], ALU.mult, ALU.add, r=[ptb, vecs_b], w=[h_b[1]])
                if last:
                    store_blk(h_t, h_b, b0, bw)
                if hook is not None:
                    hook(ctx_, t)
            end_phase()

    def p1_blkinfo(t):
        if t < 2:
            return 0, t, 256, 0, t == 1
        blk, pos = 1 + (t - 2) // 4, (t - 2) % 4
        return blk, pos, 512, 256 + (blk - 1) * 512, pos == 3

    def p1_load(ctx_, t):
        if "xt" not in ctx_:
            ctx_["xt"] = [sbt(ctx_["ph"], "p1xt%d" % i, [128, D], F32) for i in range(3)]
        x_t, x_b = ctx_["xt"][t % 3]
        k.ld(x_t[:], tok[t * 128:(t + 1) * 128, :], x_b, w=[x_b])
        return x_t, x_b

    def p1_hook(ctx_, t):
        if t % 4 == 1 and t // 4 < 8:
            blk = 4 + t // 4
            ada_load(blk + 1)
            ada_mm(blk)
            if blk == 11:
                ada_finish_B()
                if debug:
                    dv, dv_b = k.dscr("dbg_vecs", [128, 10, 16], F32)
                    k.ld(dv, vecs[:], vecs_b, r=[vecs_b], w=[dv_b])
                    dbgb.append(dv_b)

    norm_to_T("p1", NT_ALL, p1_load, lambda t: (G1C, SH1C) if t < 2 else (G1, SH1), p1_blkinfo,
              lambda h_t, h_b, b0, bw: k.ld(hT_d[:, :, b0:b0 + bw], h_t[:, :, 0:bw], h_b[0], r=list(h_b), w=[hT_b]), hook=None)
    if stop_after <= 1:
        return finish()

    class QK:
        def __init__(self, ph, name, norm_scratch=True):
            self.kw = [sbt(ph, name + "kw%d" % i, [128, 512], BF16) for i in range(2)]
            if norm_scratch:
                self.sq = [sbt(ph, name + "sq%d" % i, [128, 512], BF16) for i in range(2)]
                self.rs = [sbt(ph, name + "rs%d" % i, [128, 512], F32) for i in range(2)]
            self.t1 = [sbt(ph, name + "t1%d" % i, [128, 512], F32) for i in range(2)]
            self.t2 = [sbt(ph, name + "t2%d" % i, [128, 512], F32) for i in range(2)]
            self.i = 0

        def pre(self, praw, praw_b, bw, scale, norm):
            i = self.i
            self.i ^= 1
            kw, kw_b = self.kw[i]
            k.act(kw[:, 0:bw], praw[:, 0:bw], AF.Copy, r=[praw_b], w=[kw_b], scale=scale)
            if norm:
                sq, sq_b = self.sq[i]
                k.act(sq[:, 0:bw], praw[:, 0:bw], AF.Square, r=[praw_b], w=[sq_b])
            return i

        def post(self, i, bw, norm, cs, cs_b, out_ap, out_b, pss, prot):
            kw, kw_b = self.kw[i]
            t1, t1_b = self.t1[i]
            t2, t2_b = self.t2[i]
            if norm:
                sq, sq_b = self.sq[i]
                rs, rs_b = self.rs[i]
                k.mm(pss[0][:, 0:bw], ones, sq[:, 0:bw], True, True, r=[sq_b, cbf_b], w=[pss[1]])
                k.act(rs[:, 0:bw], pss[0][:, 0:bw], AF.Sqrt, r=[pss[1]], w=[rs_b], bias=128.0 * EPS)
                k.recip(rs[:, 0:bw], rs[:, 0:bw], r=[rs_b], w=[rs_b])
            k.mm(prot[0][:, 0:bw], pswap, kw[:, 0:bw], True, True, r=[kw_b, cbf_b], w=[prot[1]])
            k.tt(t1[:, 0:bw], kw[:, 0:bw], cs[:, 0, 0:bw], ALU.mult, r=[kw_b, cs_b], w=[t1_b])
            k.tt(t2[:, 0:bw], prot[0][:, 0:bw], cs[:, 1, 0:bw], ALU.mult, r=[prot[1], cs_b], w=[t2_b])
            if norm:
                k.tt(t1[:, 0:bw], t1[:, 0:bw], t2[:, 0:bw], ALU.add, r=[t1_b, t2_b], w=[t1_b])
                k.tt(out_ap, t1[:, 0:bw], rs[:, 0:bw], ALU.mult, r=[t1_b, rs_b], w=[out_b])
            else:
                k.tt(out_ap, t1[:, 0:bw], t2[:, 0:bw], ALU.add, r=[t1_b, t2_b], w=[out_b])

        def run(self, praw, praw_b, bw, scale, norm, cs, cs_b, out_ap, out_b, pss, prot):
            i = self.pre(praw, praw_b, bw, scale, norm)
            self.post(i, bw, norm, cs, cs_b, out_ap, out_b, pss, prot)

    def wslice(c0, n):
        return w_in[:, c0:c0 + n].rearrange("(c p) n -> p c n", p=128)

    def blk_range(blk):
        if blk == 0:
            return 0, 256
        return 256 + (blk - 1) * 512, 512

    with ExitStack() as ph:
        kT_all, kT_b = sbt(ph, "kT_all", [128, 2, NTOK], BF16)
        V_all, V_b = sbt(ph, "V_all", [128, NT_ALL, 256], BF16)
        qkw, qkw_b = sbt(ph, "qkw", [128, 2], F32)
        k.ld(qkw[:], qkw_in, qkw_b, w=[qkw_b])
        k.ts(qkw[:], qkw[:], float(np.sqrt(128.0)), None, ALU.mult, None, r=[qkw_b], w=[qkw_b])
        Wk, Wk_b = sbt(ph, "Wk", [128, 16, 256], BF16)
        Wv, Wv_b = sbt(ph, "Wv", [128, 16, 256], BF16)
        Wq, Wq_b = sbt(ph, "Wq", [128, 16, 1024], BF16)
        k.ldc(Wk[:], wslice(C_KA, 256), Wk_b, w=[Wk_b])
        k.ldc(Wv[:], wslice(C_VA, 256), Wv_b, w=[Wv_b])
        k.ldc(Wq[:], wslice(C_QA, 1024), Wq_b, w=[Wq_b])
        hb = [sbt(ph, "a_hb%d" % i, [128, 16, 512], BF16) for i in range(2)]
        cs = [sbt(ph, "a_cs%d" % i, [128, 2, 512], F32) for i in range(2)]
        qk = QK(ph, "a_")
        li = 0
        adaB = sbt(ph, "adawB", [128, 16, 1024], BF16)
        for blk in range(9):
            b0, bw = blk_range(blk)
            h_t, h_b = hb[li % 2]
            c_t, c_b = cs[li % 2]
            li += 1
            k.ld(h_t[:, :, 0:bw], hT_d[:, :, b0:b0 + bw], h_b, r=[hT_b], w=[h_b])
            k.ld(c_t[:, 0, 0:bw], cosT_in[:, b0:b0 + bw], c_b, w=[c_b])
            k.ld(c_t[:, 1, 0:bw], sinS_in[:, b0:b0 + bw], c_b, w=[c_b])
            if blk < 8:
                ada_load(4 + blk, adaB)
            iks = []
            for kvh in range(2):
                praw, praw_b = psb[0] if kvh == 0 else psb[5]
                for c in range(16):
                    k.mm(praw[:, 0:bw], Wk[:, c, kvh * 128:(kvh + 1) * 128], h_t[:, c, 0:bw], c == 0, c == 15,
                         r=[Wk_b, h_b], w=[praw_b])
                iks.append(qk.pre(praw, praw_b, bw, qkw[:, 1:2], True))
            for tl in range(bw // 128):
                t = b0 // 128 + tl
                pv, pv_b = psb[3 + (t % 2)]
                for c in range(16):
                    k.mm(pv[:, 0:256], h_t[:, c, tl * 128:(tl + 1) * 128], Wv[:, c, :], c == 0, c == 15,
                         r=[Wv_b, h_b], w=[pv_b])
                k.act(V_all[:, t, :], pv[:, 0:256], AF.Copy, r=[pv_b], w=[V_b])
            for kvh in range(2):
                qk.post(iks[kvh], bw, True, c_t, c_b, kT_all[:, kvh, b0:b0 + bw], kT_b, psb[1], psb[2])
            if blk < 8:
                ada_mm(4 + blk, adaB)
                if blk == 7:
                    ada_finish_B()
        if debug:
            d1, d1_b = k.dscr("dbg_kT", [128, 2, NTOK], BF16)
            k.ld(d1, kT_all[:], kT_b, r=[kT_b], w=[d1_b])
            d2, d2_b = k.dscr("dbg_V", [128, NT_ALL, 256], BF16)
            k.ld(d2, V_all[:], V_b, r=[V_b], w=[d2_b])
            dbgb.extend([d1_b, d2_b])
        if stop_after <= 2:
            end_phase()
            return finish()
        qblk, qblk_b = sbt(ph, "qblk", [128, 4, 512], BF16)
        ptb_ = [sbt(ph, "pt%d" % i, [128, 512], BF16) for i in range(3)]
        rden, rden_b = sbt(ph, "rden", [128, 512], F32)
        astg = [sbt(ph, "astg%d" % i, [128, 8, 512], BF16) for i in range(2)]
        sc_att = float(128.0 ** -0.5)
        for ob in range(4):
            b0 = 256 + 2048 + ob * 512
            h_t, h_b = hb[li % 2]
            c_t, c_b = cs[li % 2]
            li += 1
            k.ld(h_t[:], hT_d[:, :, b0:b0 + 512], h_b, r=[hT_b], w=[h_b])
            k.ld(c_t[:, 0, :], cosT_in[:, b0:b0 + 512], c_b, w=[c_b])
            k.ld(c_t[:, 1, :], sinS_in[:, b0:b0 + 512], c_b, w=[c_b])
            as_t, as_b = astg[ob % 2]
            for kvh in range(2):
                prev = None
                for g in range(4):
                    hd = kvh * 4 + g
                    praw, praw_b = psb[0] if g % 2 == 0 else psb[7]
                    for c in range(16):
                        k.mm(praw[:], Wq[:, c, hd * 128:(hd + 1) * 128], h_t[:, c, :], c == 0, c == 15,
                             r=[Wq_b, h_b], w=[praw_b])
                    iq = qk.pre(praw, praw_b, 512, qkw[:, 0:1], True)
                    if prev is not None:
                        qk.post(prev[0], 512, True, c_t, c_b, qblk[:, prev[1], :], qblk_b, psb[1], psb[2])
                    prev = (iq, g)
                qk.post(prev[0], 512, True, c_t, c_b, qblk[:, prev[1], :], qblk_b, psb[1], psb[2])
                for tl in range(4):
                    qsel = qblk[:, :, tl * 128:(tl + 1) * 128]
                    po, po_b = psb[5]
                    pd, pd_b = psb[6]
                    ring = [psb[3], psb[4], psb[7]]

                    def emit_st(kt, kvh=kvh, qsel=qsel, ring=ring):
                        pst, pst_b = ring[kt % 3]
                        p_t, p_b = ptb_[kt % 3]
                        k.mm(pst[:].rearrange("p (g q) -> p g q", g=4), kT_all[:, kvh, kt * 128:(kt + 1) * 128], qsel,
                             True, True, r=[kT_b, qblk_b], w=[pst_b])
                        k.act(p_t[:], pst[:], AF.Exp, r=[pst_b], w=[p_b], scale=sc_att)
                    emit_st(0)
                    emit_st(1)
                    for kt in range(NT_ALL):
                        if kt + 2 < NT_ALL:
                            emit_st(kt + 2)
                        p_t, p_b = ptb_[kt % 3]
                        k.mm(po[:], V_all[:, kt, kvh * 128:(kvh + 1) * 128], p_t[:], kt == 0, kt == NT_ALL - 1,
                             r=[V_b, p_b], w=[po_b])
                        k.mm(pd[:], ones, p_t[:], kt == 0, kt == NT_ALL - 1, r=[cbf_b, p_b], w=[pd_b])
                    k.recip(rden[:], pd[:], r=[pd_b], w=[rden_b])
                    k.tt(as_t[:, kvh * 4:(kvh + 1) * 4, tl * 128:(tl + 1) * 128],
                         po[:].rearrange("p (g q) -> p g q", g=4), rden[:].rearrange("p (g q) -> p g q", g=4),
                         ALU.mult, r=[po_b, rden_b], w=[as_b])
            k.ld(attnT_d[:, :, ob * 512:(ob + 1) * 512], as_t[:], as_b, r=[as_b], w=[attnT_b])
        end_phase()
    if stop_after <= 3:
        return finish()

    with ExitStack() as ph:
        lg, lg_b = sbt(ph, "lg", [128, 16], F32)
        k.ld(lg[:], zdec_in.partition_broadcast(128), lg_b, w=[lg_b])
        k.act(lg[:], lg[:], AF.Exp, r=[lg_b], w=[lg_b])
        k.ts(lg[:], lg[:], -1.0, None, ALU.mult, None, r=[lg_b], w=[lg_b])
        kpos, kpos_b = sbt(ph, "kpos", [128, 16], F32)
        k.ld(kpos[:], kpos_in, kpos_b, w=[kpos_b])
        dist, dist_b = sbt(ph, "dist", [128, 2, NT_ALL], F32)
        k.ld(dist[:], dist_in, dist_b, w=[dist_b])
        Dk, Dk_b = sbt(ph, "Dk", [128, 16], F32)
        k.tt(Dk[:], kpos[:], lg[:], ALU.mult, r=[kpos_b, lg_b], w=[Dk_b])
        k.act(Dk[:], Dk[:], AF.Exp, r=[Dk_b], w=[Dk_b])
        npos, npos_b = sbt(ph, "npos", [128, 2, 16], F32)
        k.ld(npos[:], npos_in, npos_b, w=[npos_b])
        Kdec, Kdec_b = sbt(ph, "Kdec", [128, 2, 16], F32)
        qp_t, qp_b = sbt(ph, "qp_t", [128, 2, 512], F32)
        qsc, qsc_b = sbt(ph, "qsc", [128, 2, 512], F32)
        MT, MT_b = sbt(ph, "MT", [128, 8, 128], F32)
        e1, e1_b = sbt(ph, "e1", [128, 128], F32)
        e2, e2_b = sbt(ph, "e2", [128, 128], F32)
        for h in range(8):
            k.act(e1[:], cmat[:, 2, :], AF.Exp, r=[cmat_b, lg_b], w=[e1_b], scale=lg[:, h:h + 1])
            k.tt(e1[:], e1[:], cmat[:, 3, :], ALU.mult, r=[e1_b, cmat_b], w=[e1_b])
            k.act(e2[:], cmat[:, 4, :], AF.Exp, r=[cmat_b, lg_b], w=[e2_b], scale=lg[:, 8 + h:9 + h])
            k.tt(e2[:], e2[:], cmat[:, 5, :], ALU.mult, r=[e2_b, cmat_b], w=[e2_b])
            k.tt(MT[:, h, :], e1[:], e2[:], ALU.add, r=[e1_b, e2_b], w=[MT_b])
        rnw, rnw_b = sbt(ph, "rnw", [128, D], F32)
        k.ld(rnw[:], rnw_in.partition_broadcast(128), rnw_b, w=[rnw_b])
        Wq_h, Wq_hb = sbt(ph, "Wq_h", [128, 16, 128], BF16)
        Wk_h, Wk_hb = sbt(ph, "Wk_h", [128, 16, 128], BF16)
        Wv_h, Wv_hb = sbt(ph, "Wv_h", [128, 16, 256], BF16)
        Wg_h, Wg_hb = sbt(ph, "Wg_h", [128, 16, 256], BF16)
        hb = [sbt(ph, "r_hb%d" % i, [128, 16, 512], BF16) for i in range(2)]
        cs = [sbt(ph, "r_cs%d" % i, [128, 2, 512], F32) for i in range(2)]
        qk = QK(ph, "r_", norm_scratch=False)
        ktb = [sbt(ph, "ktb%d" % i, [128, 512], BF16) for i in range(2)]
        v_all, v_b = sbt(ph, "v_all", [128, NT_ALL, 256], BF16)
        kpre, kpre_b = sbt(ph, "kpre", [128, 2, 18, 128], BF16)
        kdf, kdf_b = sbt(ph, "kdf", [128, 16, 128], BF16)
        kdb, kdb_b = sbt(ph, "kdb", [128, 16, 128], BF16)
        qT_own, qT_b = sbt(ph, "qT_own", [128, 2048], BF16)
        qf, qf_b = sbt(ph, "qf", [128, 2048], BF16)
        qbk, qbk_b = sbt(ph, "qbk", [128, 2048], BF16)
        kT_own, kTo_b = sbt(ph, "kT_own", [128, 2048], BF16)
        g_own, g_b = sbt(ph, "g_own", [128, 16, 256], BF16)
        o_acc, o_b = sbt(ph, "o_acc", [128, 16, 256], F32)
        o_bufs = [Buf("o_acc%d" % j) for j in range(16)]
        SbfF = [sbt(ph, "SbfF%d" % i, [128, 256], BF16) for i in range(2)]
        SbfB = [sbt(ph, "SbfB%d" % i, [128, 256], BF16) for i in range(2)]
        AT = [sbt(ph, "AT%d" % i, [128, 128], BF16) for i in range(2)]
        junk2, junk2_b = sbt(ph, "junk2", [128, 256], F32)
        ytmp = [sbt(ph, "ytmp%d" % i, [128, 256], F32) for i in range(2)]
        ybf = [sbt(ph, "ybf%d" % i, [128, 256], BF16) for i in range(4)]
        hst = [sbt(ph, "hst%d" % i, [128, 8], F32) for i in range(2)]
        wpre, wpre_b = sbt(ph, "wpre", [128, 2, NT_ALL], F32)
        rstage, rst_b = sbt(ph, "rstage", [128, 2, 2048], BF16)
        li = 0
        rk_scale = float(128.0 ** -0.5)
        pending_norm = []

        def drain_norm(n):
            for _ in range(n):
                if pending_norm:
                    for f in pending_norm.pop(0):
                        f()
        for h in range(8):
            k.ldc(Wq_h[:], wslice(C_QR + h * 128, 128), Wq_hb, w=[Wq_hb])
            k.ldc(Wk_h[:], wslice(C_KR + h * 128, 128), Wk_hb, w=[Wk_hb])
            k.ldc(Wv_h[:], wslice(C_VR + h * 256, 256), Wv_hb, w=[Wv_hb])
            k.ldc(Wg_h[:], wslice(C_GR + h * 256, 256), Wg_hb, w=[Wg_hb])
            for d in range(2):
                k.act(wpre[:, d, :], dist[:, d, :], AF.Exp, r=[dist_b, lg_b], w=[wpre_b], scale=lg[:, d * 8 + h:d * 8 + h + 1])
                k.act(Kdec[:, d, :], npos[:, d, :], AF.Exp, r=[npos_b, lg_b], w=[Kdec_b], scale=lg[:, d * 8 + h:d * 8 + h + 1])
            for blk in range(9):
                b0, bw = blk_range(blk)
                own = blk >= 5
                ob = blk - 5
                h_t, h_b = hb[li % 2]
                c_t, c_b = cs[li % 2]
                kt_t, kt_b = ktb[li % 2]
                li += 1
                k.ld(h_t[:, :, 0:bw], hT_d[:, :, b0:b0 + bw], h_b, r=[hT_b], w=[h_b])
                k.ld(c_t[:, 0, 0:bw], cosT_in[:, b0:b0 + bw], c_b, w=[c_b])
                k.ld(c_t[:, 1, 0:bw], sinS_in[:, b0:b0 + bw], c_b, w=[c_b])
                praw, praw_b = psb[0]
                for c in range(16):
                    k.mm(praw[:, 0:bw], Wk_h[:, c, :], h_t[:, c, 0:bw], c == 0, c == 15, r=[Wk_hb, h_b], w=[praw_b])
                if own:
                    kout, kout_b = kT_own[:, ob * 512:(ob + 1) * 512], kTo_b
                    kfull = kT_own
                    koff = ob * 512
                else:
                    kout, kout_b = kt_t[:, 0:bw], kt_b
                    kfull = kt_t
                    koff = 0
                ik = qk.pre(praw, praw_b, bw, rk_scale, False)
                ntl = bw // 128
                for tl in range(ntl):
                    t = b0 // 128 + tl
                    pv, pv_b = psb[2 + (t % 2)]
                    for c in range(16):
                        k.mm(pv[:, 0:256], h_t[:, c, tl * 128:(tl + 1) * 128], Wv_h[:, c, :], c == 0, c == 15,
                             r=[Wv_hb, h_b], w=[pv_b])
                    k.act(v_all[:, t, :], pv[:, 0:256], AF.Copy, r=[pv_b], w=[v_b])
                qk.post(ik, bw, False, c_t, c_b, kout, kout_b, None, psb[1])
                if own:
                    praw, praw_b = psb[0]
                    for c in range(16):
                        k.mm(praw[:], Wq_h[:, c, :], h_t[:, c, :], c == 0, c == 15, r=[Wq_hb, h_b], w=[praw_b])
                    qs = slice(ob * 512, (ob + 1) * 512)
                    iq = qk.pre(praw, praw_b, 512, 1.0, False)
                    for tl in range(4):
                        j = ob * 4 + tl
                        pg, pg_b = psb[6 + (tl % 2)]
                        for c in range(16):
                            k.mm(pg[:, 0:256], h_t[:, c, tl * 128:(tl + 1) * 128], Wg_h[:, c, :], c == 0, c == 15,
                                 r=[Wg_hb, h_b], w=[pg_b])
                        k.act(g_own[:, j, :], pg[:, 0:256], AF.Silu, r=[pg_b], w=[g_b])
                    qk.post(iq, 512, False, c_t, c_b, qT_own[:, qs], qT_b, None, psb[1])
                    k.ld(qp_t[:], qpos_in[:, :, qs], qp_b, w=[qp_b])
                    for d in range(2):
                        k.act(qsc[:, d, :], qp_t[:, d, :], AF.Exp, r=[qp_b, lg_b], w=[qsc_b], scale=lg[:, d * 8 + h:d * 8 + h + 1])
                    k.tt(qf[:, qs], qT_own[:, qs], qsc[:, 0, :], ALU.mult, r=[qT_b, qsc_b], w=[qf_b])
                    k.tt(qbk[:, qs], qT_own[:, qs], qsc[:, 1, :], ALU.mult, r=[qT_b, qsc_b], w=[qbk_b])
                for tl in range(ntl):
                    t = b0 // 128 + tl
                    ptr, ptr_b = psb[4 + (t % 2)]
                    ptv = ptr[:].bitcast(BF16)
                    k.tr(ptv[:, 0:128], kfull[:, koff + tl * 128:koff + (tl + 1) * 128], ident, r=[kout_b, cbf_b], w=[ptr_b])
                    if t < 18:
                        for d in range(2):
                            k.act(kpre[:, d, t, :], ptv[:, 0:128], AF.Copy, r=[ptr_b, wpre_b], w=[kpre_b],
                                  scale=wpre[:, d, t:t + 1])
                    else:
                        j = t - 18
                        k.act(kdf[:, j, :], ptv[:, 0:128], AF.Copy, r=[ptr_b, Kdec_b], w=[kdf_b], scale=Kdec[:, 0, j:j + 1])
                        k.act(kdb[:, j, :], ptv[:, 0:128], AF.Copy, r=[ptr_b, Kdec_b], w=[kdb_b], scale=Kdec[:, 1, j:j + 1])
                if blk <= 4:
                    drain_norm(1)
            pSf, pSf_b = psb[6]
            pSb, pSb_b = psb[7]
            for t in range(18):
                k.mm(pSf[:, 0:256], kpre[:, 0, t, :], v_all[:, t, :], t == 0, True, r=[kpre_b, v_b], w=[pSf_b])
                k.mm(pSb[:, 0:256], kpre[:, 1, t, :], v_all[:, t, :], t == 0, True, r=[kpre_b, v_b], w=[pSb_b])
            k.act(SbfF[0][0][:], pSf[:, 0:256], AF.Copy, r=[pSf_b], w=[SbfF[0][1]])
            k.act(SbfB[0][0][:], pSb[:, 0:256], AF.Copy, r=[pSb_b], w=[SbfB[0][1]])
            for s in range(16):
                j = s
                js = slice(j * 128, (j + 1) * 128)
                pst, pst_b = psb[0]
                k.mm(pst[:, 0:128], kT_own[:, js], qT_own[:, js], True, True, r=[kTo_b, qT_b], w=[pst_b])
                a_t, a_b = AT[s % 2]
                k.tt(a_t[:], pst[:, 0:128], MT[:, h, :], ALU.mult, r=[pst_b, MT_b], w=[a_b])
                po, po_b = psb[1 + (s % 2)]
                sf_t, sf_b = SbfF[s % 2]
                k.mm(po[:, 0:256], qf[:, js], sf_t[:], True, False, r=[qf_b, sf_b], w=[po_b])
                k.mm(po[:, 0:256], a_t[:], v_all[:, 18 + j, :], False, True, r=[a_b, v_b], w=[po_b])
                if s < 8:
                    k.act(o_acc[:, j, :], po[:, 0:256], AF.Copy, r=[po_b], w=[o_bufs[j]])
                else:
                    k.tt(o_acc[:, j, :], o_acc[:, j, :], po[:, 0:256], ALU.add, r=[o_bufs[j], po_b], w=[o_bufs[j]])
                if s < 15:
                    k.mm(pSf[:, 0:256], kdf[:, j, :], v_all[:, 18 + j, :], False, True, r=[kdf_b, v_b], w=[pSf_b])
                    nf_t, nf_b = SbfF[(s + 1) % 2]
                    k.act(nf_t[:], pSf[:, 0:256], AF.Copy, r=[pSf_b], w=[nf_b])
                j = 15 - s
                js = slice(j * 128, (j + 1) * 128)
                pc, pc_b = psb[4 + (s % 2)]
                sb_t, sb_b = SbfB[s % 2]
                k.mm(pc[:, 0:256], qbk[:, js], sb_t[:], True, True, r=[qbk_b, sb_b], w=[pc_b])
                if s < 8:
                    k.act(o_acc[:, j, :], pc[:, 0:256], AF.Copy, r=[pc_b], w=[o_bufs[j]])
                else:
                    k.tt(o_acc[:, j, :], o_acc[:, j, :], pc[:, 0:256], ALU.add, r=[o_bufs[j], pc_b], w=[o_bufs[j]])
                if s < 15:
                    k.mm(pSb[:, 0:256], kdb[:, j, :], v_all[:, 18 + j, :], False, True, r=[kdb_b, v_b], w=[pSb_b])
                    nb_t, nb_b = SbfB[(s + 1) % 2]
                    k.act(nb_t[:], pSb[:, 0:256], AF.Copy, r=[pSb_b], w=[nb_b])
            def mk_part1(j0, h=h):
                def part1():
                    jj = (j0, j0 + 1)
                    O = [o_acc[:, j, :] for j in jj]
                    OB = [o_bufs[j] for j in jj]
                    ST_ = [hst[j % 2] for j in jj]
                    YT = [ytmp[j % 2] for j in jj]
                    YB = [ybf[j % 4] for j in jj]
                    for i in range(2):
                        k.act(junk2[:], O[i], AF.Copy, r=[OB[i]], w=[junk2_b, ST_[i][1]], accum_out=ST_[i][0][:, 0:1])
                    for i in range(2):
                        k.act(junk2[:], O[i], AF.Square, r=[OB[i]], w=[junk2_b, ST_[i][1]], accum_out=ST_[i][0][:, 1:2])
                    for i in range(2):
                        s_t, s_b = ST_[i]
                        k.ts(s_t[:, 2:3], s_t[:, 0:1], 1.0 / 256, None, ALU.mult, None, r=[s_b], w=[s_b])
                    for i in range(2):
                        s_t, s_b = ST_[i]
                        k.tt(s_t[:, 3:4], s_t[:, 2:3], s_t[:, 2:3], ALU.mult, r=[s_b], w=[s_b])
                    for i in range(2):
                        s_t, s_b = ST_[i]
                        k.stt(s_t[:, 4:5], s_t[:, 1:2], 1.0 / 256, s_t[:, 3:4], ALU.mult, ALU.subtract, r=[s_b], w=[s_b])
                    for i in range(2):
                        s_t, s_b = ST_[i]
                        k.act(s_t[:, 5:6], s_t[:, 4:5], AF.Sqrt, r=[s_b], w=[s_b], bias=EPS)
                    for i in range(2):
                        s_t, s_b = ST_[i]
                        k.recip(s_t[:, 5:6], s_t[:, 5:6], r=[s_b], w=[s_b])
                    for i in range(2):
                        s_t, s_b = ST_[i]
                        k.ts(YT[i][0][:], O[i], s_t[:, 2:3], s_t[:, 5:6], ALU.subtract, ALU.mult, r=[OB[i], s_b], w=[YT[i][1]])
                    for i in range(2):
                        k.tt(YT[i][0][:], YT[i][0][:], rnw[:, h * 256:(h + 1) * 256], ALU.mult, r=[YT[i][1], rnw_b], w=[YT[i][1]])
                    for i in range(2):
                        k.tt(YB[i][0][:], YT[i][0][:], g_own[:, jj[i], :], ALU.mult, r=[YT[i][1], g_b], w=[YB[i][1]])
                return part1

            def mk_part2(j0):
                def part2():
                    for j in (j0, j0 + 1):
                        yb_t, yb_b = ybf[j % 4]
                        ptr, ptr_b = psb[4 + (j % 2)]
                        ptv = ptr[:].bitcast(BF16)
                        k.tr(ptv[:, 0:128], yb_t[:, 0:128], ident, r=[yb_b, cbf_b], w=[ptr_b])
                        k.tr(ptv[:, 128:256], yb_t[:, 128:256], ident, r=[yb_b, cbf_b], w=[ptr_b])
                        k.act(rstage[:, :, j * 128:(j + 1) * 128], ptv[:, 0:256].rearrange("p (a b) -> p a b", a=2), AF.Copy,
                              r=[ptr_b], w=[rst_b])
                return part2

            def mk_store(h=h):
                def store():
                    k.ld(retT_d[:, 2 * h:2 * h + 2, :], rstage[:], rst_b, r=[rst_b], w=[retT_b])
                return store
            P1 = [mk_part1(j0) for j0 in range(0, 16, 2)]
            P2 = [mk_part2(j0) for j0 in range(0, 16, 2)]
            pending_norm.extend([P1[0:2], P2[0:2] + P1[2:4], P2[2:4] + P1[4:6], P2[4:6] + P1[6:8], P2[6:8] + [mk_store()]])
        drain_norm(len(pending_norm))
        end_phase()
    if stop_after <= 4:
        return finish()

    with ExitStack() as ph:
        Wab_n, Wab_b = sbt(ph, "Wab_n", [128, 8, 512], BF16)
        Wrb_n, Wrb_b = sbt(ph, "Wrb_n", [128, 16, 512], BF16)
        Wga_n, Wga_b = sbt(ph, "Wga_n", [128, 16, 512], BF16)
        Wgb_n, Wgb_b = sbt(ph, "Wgb_n", [128, 16, 512], BF16)
        abk = [sbt(ph, "m_ab%d" % i, [128, 8, 512], BF16) for i in range(2)]
        rbk = [sbt(ph, "m_rb%d" % i, [128, 16, 512], BF16) for i in range(2)]
        hbk = [sbt(ph, "m_hb%d" % i, [128, 16, 512], BF16) for i in range(2)]
        sga = [sbt(ph, "sga%d" % i, [128, 512], F32) for i in range(2)]
        sgb = [sbt(ph, "sgb%d" % i, [128, 512], F32) for i in range(2)]
        m1 = [sbt(ph, "m1%d" % i, [128, 512], F32) for i in range(2)]
        m2 = [sbt(ph, "m2%d" % i, [128, 512], F32) for i in range(2)]
        mixb = [sbt(ph, "mixb%d" % i, [128, 512], BF16) for i in range(2)]
        mstage = [sbt(ph, "mstage%d" % i, [128, 4, 512], BF16) for i in range(2)]
        li = 0
        pend5 = []
        for nb in range(4):
            ns = slice(nb * 512, (nb + 1) * 512)
            k.ldc(Wab_n[:], w_ab[:, ns].rearrange("(c p) n -> p c n", p=128), Wab_b, w=[Wab_b])
            k.ldc(Wrb_n[:], w_rb[:, ns].rearrange("(c p) n -> p c n", p=128), Wrb_b, w=[Wrb_b])
            k.ldc(Wga_n[:], wslice(C_GA + nb * 512, 512), Wga_b, w=[Wga_b])
            k.ldc(Wgb_n[:], wslice(C_GB + nb * 512, 512), Wgb_b, w=[Wgb_b])
            for ob in range(4):
                a_t, a_b = abk[li % 2]
                r_t, r_b = rbk[li % 2]
                h_t, h_b = hbk[li % 2]
                ms_t, ms_b = mstage[li % 2]
                li += 1
                os_ = slice(ob * 512, (ob + 1) * 512)
                k.ld(a_t[:], attnT_d[:, :, os_], a_b, r=[attnT_b], w=[a_b])
                k.ld(r_t[:], retT_d[:, :, os_], r_b, r=[retT_b], w=[r_b])
                k.ld(h_t[:], hT_d[:, :, 2304 + ob * 512:2304 + (ob + 1) * 512], h_b, r=[hT_b], w=[h_b])
                for tl in range(4):
                    ts_ = slice(tl * 128, (tl + 1) * 128)
                    i2 = tl % 2
                    base = 4 * (tl % 2)
                    pGa, pGa_b = psb[base + 0]
                    pGb, pGb_b = psb[base + 1]
                    pA, pA_b = psb[base + 2]
                    pR, pR_b = psb[base + 3]
                    for c in range(16):
                        k.mm(pGa[:], h_t[:, c, ts_], Wga_n[:, c, :], c == 0, c == 15, r=[h_b, Wga_b], w=[pGa_b])
                    k.act(sga[i2][0][:], pGa[:], AF.Sigmoid, r=[pGa_b], w=[sga[i2][1]])
                    for c in range(16):
                        k.mm(pGb[:], h_t[:, c, ts_], Wgb_n[:, c, :], c == 0, c == 15, r=[h_b, Wgb_b], w=[pGb_b])
                    k.act(sgb[i2][0][:], pGb[:], AF.Sigmoid, r=[pGb_b], w=[sgb[i2][1]])
                    for hh in range(8):
                        k.mm(pA[:], a_t[:, hh, ts_], Wab_n[:, hh, :], hh == 0, hh == 7, r=[a_b, Wab_b], w=[pA_b])
                    k.tt(m1[i2][0][:], pA[:], sga[i2][0][:], ALU.mult, r=[pA_b, sga[i2][1]], w=[m1[i2][1]])
                    for c in range(16):
                        k.mm(pR[:], r_t[:, c, ts_], Wrb_n[:, c, :], c == 0, c == 15, r=[r_b, Wrb_b], w=[pR_b])
                    k.tt(m2[i2][0][:], pR[:], sgb[i2][0][:], ALU.mult, r=[pR_b, sgb[i2][1]], w=[m2[i2][1]])
                    k.tt(mixb[i2][0][:], m1[i2][0][:], m2[i2][0][:], ALU.add, r=[m1[i2][1], m2[i2][1]], w=[mixb[i2][1]])
                    def fin(pGa=pGa, pGa_b=pGa_b, i2=i2, ms_t=ms_t, ms_b=ms_b, ts_=ts_, tl=tl, nb=nb, os_=os_):
                        ptv = pGa[:].bitcast(BF16)
                        for cc in range(4):
                            k.tr(ptv[:, cc * 128:(cc + 1) * 128], mixb[i2][0][:, cc * 128:(cc + 1) * 128], ident,
                                 r=[mixb[i2][1], cbf_b], w=[pGa_b])
                        k.act(ms_t[:, :, ts_], ptv[:, 0:512].rearrange("p (a b) -> p a b", a=4), AF.Copy, r=[pGa_b], w=[ms_b])
                        if tl == 3:
                            k.ld(mixT_d[:, nb * 4:(nb + 1) * 4, os_], ms_t[:], ms_b, r=[ms_b], w=[mixT_b])
                    if pend5:
                        pend5.pop()()
                    pend5.append(fin)
        pend5.pop()()
        end_phase()
    if stop_after <= 5:
        return finish()

    def load_grow(ph, nm):
        grow, grow_b = sbt(ph, nm, [128, 3, D], F32)
        k.ld(grow[:, 0, :], gsc_d[0].partition_broadcast(128), grow_b, r=[gsc_b], w=[grow_b])
        k.ld(grow[:, 1, :], gsc_d[1].partition_broadcast(128), grow_b, r=[gsc_b], w=[grow_b])
        k.ld(grow[:, 2, :], nfw_in.partition_broadcast(128), grow_b, w=[grow_b])
        return grow, grow_b

    def p6_load(ctx_, t):
        ph = ctx_["ph"]
        if "Wmo" not in ctx_:
            ctx_["grow"] = load_grow(ph, "grow6")
            ctx_["Wmo"] = sbt(ph, "Wmo", [128, 16, D], BF16)
            for nb in range(4):
                k.ldc(ctx_["Wmo"][0][:, :, nb * 512:(nb + 1) * 512],
                      w_mo[:, nb * 512:(nb + 1) * 512].rearrange("(c p) n -> p c n", p=128), ctx_["Wmo"][1], w=[ctx_["Wmo"][1]])
            ctx_["mb"] = [sbt(ph, "p6mb%d" % i, [128, 16, 512], BF16) for i in range(2)]
            ctx_["xt"] = [sbt(ph, "p6xt%d" % i, [128, D], F32) for i in range(2)]
            ctx_["x1"] = [sbt(ph, "p6x1%d" % i, [128, D], F32) for i in range(2)]
        Wmo, Wmo_b = ctx_["Wmo"]
        grow, grow_b = ctx_["grow"]
        ob, tl = divmod(t, 4)
        m_t, m_b = ctx_["mb"][ob % 2]
        if tl == 0:
            k.ld(m_t[:], mixT_d[:, :, ob * 512:(ob + 1) * 512], m_b, r=[mixT_b], w=[m_b])
        x_t, x_b = ctx_["xt"][t % 2]
        x1_t, x1t_b = ctx_["x1"][t % 2]
        k.ld(x_t[:], tok[(OWN0 + t) * 128:(OWN0 + t + 1) * 128, :], x_b, w=[x_b])
        for nb in range(4):
            ns = slice(nb * 512, (nb + 1) * 512)
            py, py_b = psb[4 + nb]
            for c in range(16):
                k.mm(py[:], m_t[:, c, tl * 128:(tl + 1) * 128], Wmo[:, c, ns], c == 0, c == 15, r=[m_b, Wmo_b], w=[py_b])
            k.tt(x1_t[:, ns], py[:], grow[:, 0, ns], ALU.mult, r=[py_b, grow_b], w=[x1t_b])
            k.tt(x1_t[:, ns], x1_t[:, ns], x_t[:, ns], ALU.add, r=[x1t_b, x_b], w=[x1t_b])
        k.ld(x1_d[t * 128:(t + 1) * 128, :], x1_t[:], x1t_b, r=[x1t_b], w=[x1_b])
        return x1_t, x1t_b

    norm_to_T("p6", NOWN, p6_load, lambda t: (G2, SH2), lambda t: (t // 4, t % 4, 512, (t // 4) * 512, t % 4 == 3),
              lambda h_t, h_b, b0, bw: k.ld(h2T_d[:, :, b0:b0 + bw], h_t[:, :, 0:bw], h_b[0], r=list(h_b), w=[h2T_b]))
    if stop_after <= 6:
        return finish()

    with ExitStack() as ph:
        Wpq, Wpq_b = sbt(ph, "Wpq", [128, 16, D], BF16)
        for nb in range(4):
            k.ldc(Wpq[:, :, nb * 512:(nb + 1) * 512], pwq[:, nb * 512:(nb + 1) * 512].rearrange("(c p) n -> p c n", p=128),
                  Wpq_b, w=[Wpq_b])
        h2a = [sbt(ph, "p7ah%d" % i, [128, 16, 512], BF16) for i in range(2)]
        qst = [sbt(ph, "p7aq%d" % i, [128, 16, 512], BF16) for i in range(2)]
        for ob in range(4):
            h_t, h_b = h2a[ob % 2]
            q_t, q_b = qst[ob % 2]
            k.ld(h_t[:], h2T_d[:, :, ob * 512:(ob + 1) * 512], h_b, r=[h2T_b], w=[h_b])
            for hp in range(16):
                pq, pq_b = psb[hp % 4]
                for c in range(16):
                    k.mm(pq[:], Wpq[:, c, hp * 128:(hp + 1) * 128], h_t[:, c, :], c == 0, c == 15, r=[Wpq_b, h_b], w=[pq_b])
                if hp % 2 == 0:
                    k.act(q_t[:, hp, :], pq[:], AF.Copy, r=[pq_b], w=[q_b])
                else:
                    tcopy(q_t[:, hp, :], pq[:], r=[pq_b], w=[q_b])
            k.ld(qpT_d[:, :, ob * 512:(ob + 1) * 512], q_t[:], q_b, r=[q_b], w=[qpTd_b])
        end_phase()

    with ExitStack() as ph:
        keys, keys_b = sbt(ph, "keys", [128, 16, 128], BF16)
        k.ldc(keys[:], keysT_in, keys_b, w=[keys_b])
        iot, iot_b = sbt(ph, "iot", [128, 512], F32)
        k.ld(iot[:], iota_in, iot_b, w=[iot_b])
        iof = iot[:, 0:128]
        io256 = iot[:, 128:384].bitcast(I32)
        io128 = iot[:, 384:512].bitcast(I32)
        qpT, qpT_b = sbt(ph, "qpT", [128, 16, 128], BF16)
        h2p = [sbt(ph, "p7h%d" % i, [128, 16, 512], BF16) for i in range(4)]
        uGp = [sbt(ph, "p7u%d" % i, [128, 16, 512], BF16) for i in range(2)]
        gst = [sbt(ph, "p7g%d" % i, [128, 4, 512], BF16) for i in range(2)]
        ga_dv = ga_d.rearrange("c i t -> i c t")
        pre_banks = [psb[5], psb[6], psb[7]]

        def pre_units():
            nu = 0
            npb = 0
            for i in range(4):
                k.ld(h2p[i][0][:], h2T_d[:, :, i * 512:(i + 1) * 512], h2p[i][1], r=[h2T_b], w=[h2p[i][1]])
            for G in range(32):
                c0 = G * 4
                u_t, u_b = uGp[G % 2]
                k.ldc(u_t[:].rearrange("p k (c i) -> p k c i", c=4),
                      uT[:, c0:c0 + 4, :].rearrange("(k p) c i -> p k c i", p=128), u_b, w=[u_b])
                for tb in range(4):
                    g_t, g_b2 = gst[nu % 2]
                    nu += 1
                    for cc in range(4):
                        pa, pa_b = pre_banks[npb % 3]
                        npb += 1
                        for kk in range(16):
                            k.mm(pa[:], u_t[:, kk, cc * 128:(cc + 1) * 128], h2p[tb][0][:, kk, :], kk == 0, kk == 15,
                                 r=[u_b, h2p[tb][1]], w=[pa_b])
                        k.act(g_t[:, cc, :], pa[:], AF.Gelu_apprx_tanh, r=[pa_b], w=[g_b2])
                        if cc == 3:
                            k.ld(ga_dv[:, c0:c0 + 4, tb * 512:(tb + 1) * 512], g_t[:], g_b2, r=[g_b2], w=[ga_b])
                        yield
        pre_gen = pre_units()

        def pre_steps(n):
            for _ in range(n):
                next(pre_gen, None)
        s_sb, s_b = sbt(ph, "s_sb", [128, 16, 128], F32)
        scr, scr_b = sbt(ph, "scr", [128, 4, 128], F32)
        top, top_b = sbt(ph, "top", [128, 16, 16], F32)
        idxi, idxi_b = sbt(ph, "idxi", [128, 16, 16], I32)
        idxf, idxf_b = sbt(ph, "idxf", [128, 16, 16], F32)
        cand, cand_b = sbt(ph, "cand", [128, 8, 256], F32)
        cscr, cscr_b = sbt(ph, "cscr", [128, 4, 256], F32)
        ctop, ctop_b = sbt(ph, "ctop", [128, 8, 16], F32)
        sm, sm_b = sbt(ph, "sm", [128, 3, 8], F32)
        ee, ee_b = sbt(ph, "ee", [128, 8, 16], F32)
        ci, ci_b = sbt(ph, "ci", [128, 3, 8, 16], I32)
        cf, cf_b = sbt(ph, "cf", [128, 2, 8, 16], F32)
        abg, abg_b = sbt(ph, "abg", [128, 3, 8, 16], F32)
        abgT = [sbt(ph, "abgT%d" % i, [128, 3, 128], F32) for i in range(2)]
        NB_OH = 4
        At = [sbt(ph, "At%d" % i, [128, 8, 128], BF16) for i in range(NB_OH)]
        iob16, iob_b = sbt(ph, "iob16", [128, 128], BF16)
        tcopy(iob16[:], iof, r=[iot_b], w=[iob_b])
        aTb, aTb_b = sbt(ph, "aTb", [128, 128], BF16)
        gGt = [sbt(ph, "gGt%d" % i, [128, 8, 128], BF16) for i in range(NB_OH)]
        Wst = [sbt(ph, "Wst%d" % i, [128, 128, 128], BF16) for i in range(1)]
        W_dv = W_d.rearrange("c i t -> i c t")
        pwb = [psb[0], psb[1]]
        pwctr = [0]
        noh = 0
        def score_mm(hp, ts_):
            pss_, pss_b = psb[2 + (hp // 4) % 2]
            k.mm(pss_[:, (hp % 4) * 128:(hp % 4 + 1) * 128], qpT[:, hp, :], keys[:, hp, :], True, True,
                 r=[qpT_b, keys_b], w=[pss_b])

        def load_qp(t):
            k.ld(qpT[:], qpT_d[:, :, t * 128:(t + 1) * 128], qpT_b, r=[qpTd_b], w=[qpT_b])
        pre_issued = False
        for ob in range(4):
            for tl in range(4):
                t = ob * 4 + tl
                ts_ = slice(tl * 128, (tl + 1) * 128)
                if not pre_issued:
                    load_qp(t)
                for hp in range(16):
                    pss_, pss_b = psb[2 + (hp // 4) % 2]
                    if not (pre_issued and hp < 8):
                        score_mm(hp, ts_)
                    if hp % 4 == 3:
                        bk = hp // 4
                        dsti = s_sb[:, bk * 4:(bk + 1) * 4, :].rearrange("p a b -> p (a b)").bitcast(I32)
                        srci = pss_[:].bitcast(I32)
                        fw.op("dve", lambda dsti=dsti, srci=srci: nc.vector.tensor_single_scalar(
                            out=dsti, in_=srci, scalar=-128, op=ALU.bitwise_and), r=[pss_b], w=[s_b])
                s_i = s_sb[:].bitcast(I32)
                k.tt(s_i, s_i, io128.unsqueeze(1).to_broadcast([128, 16, 128]), ALU.bitwise_or, r=[s_b, iot_b], w=[s_b])
                pre_issued = False
                pre_steps(4)
                IL = 4
                for hp0 in range(0, 16, IL):
                    for hp in range(hp0, hp0 + IL):
                        fw.op("dve", lambda hp=hp: nc.vector.max(out=top[:, hp, 0:8], in_=s_sb[:, hp, :]), r=[s_b], w=[top_b])
                    for hp in range(hp0, hp0 + IL):
                        fw.op("dve", lambda hp=hp: nc.vector.match_replace(out=scr[:, hp % IL, :], in_to_replace=top[:, hp, 0:8],
                                                                          in_values=s_sb[:, hp, :], imm_value=-1e30),
                              r=[s_b, top_b], w=[scr_b])
                    for hp in range(hp0, hp0 + IL):
                        fw.op("dve", lambda hp=hp: nc.vector.max(out=top[:, hp, 8:16], in_=scr[:, hp % IL, :]), r=[scr_b], w=[top_b])
                fw.op("dve", lambda: nc.vector.tensor_single_scalar(out=idxi[:], in_=top[:].bitcast(I32), scalar=127,
                                                                   op=ALU.bitwise_and), r=[top_b], w=[idxi_b])
                tcopy(idxf[:], idxi[:], r=[idxi_b], w=[idxf_b])
                pre_steps(3)
                topv = top[:].rearrange("p (h two) k -> p h two k", two=2)
                idxv = idxf[:].rearrange("p (h two) k -> p h two k", two=2)
                cand4 = cand[:].rearrange("p h (i j) -> p h i j", i=16)
                k.tt(cand4, topv[:, :, 0, :].unsqueeze(3).to_broadcast([128, 8, 16, 16]),
                     topv[:, :, 1, :].unsqueeze(2).to_broadcast([128, 8, 16, 16]), ALU.add, r=[top_b], w=[cand_b])
                candi = cand[:].bitcast(I32)
                fw.op("dve", lambda candi=candi: nc.vector.tensor_single_scalar(out=candi, in_=candi, scalar=-256,
                                                                               op=ALU.bitwise_and), r=[cand_b], w=[cand_b])
                k.tt(candi, candi, io256.unsqueeze(1).to_broadcast([128, 8, 256]), ALU.bitwise_or, r=[cand_b, iot_b], w=[cand_b])
                pre_steps(3)
                for h0 in range(0, 8, IL):
                    for h in range(h0, h0 + IL):
                        fw.op("dve", lambda h=h: nc.vector.max(out=ctop[:, h, 0:8], in_=cand[:, h, :]), r=[cand_b], w=[ctop_b])
                    for h in range(h0, h0 + IL):
                        fw.op("dve", lambda h=h: nc.vector.match_replace(out=cscr[:, h % IL, :], in_to_replace=ctop[:, h, 0:8],
                                                                        in_values=cand[:, h, :], imm_value=-1e30),
                              r=[cand_b, ctop_b], w=[cscr_b])
                    for h in range(h0, h0 + IL):
                        fw.op("dve", lambda h=h: nc.vector.max(out=ctop[:, h, 8:16], in_=cscr[:, h % IL, :]), r=[cscr_b], w=[ctop_b])
                k.ts(sm[:, 0, :], ctop[:, :, 0], -1.0, None, ALU.mult, None, r=[ctop_b], w=[sm_b])
                for h in range(8):
                    k.act(ee[:, h, :], ctop[:, h, :], AF.Exp, r=[ctop_b, sm_b], w=[ee_b, sm_b], bias=sm[:, 0, h:h + 1],
                          accum_out=sm[:, 1, h:h + 1])
                k.recip(sm[:, 2, :], sm[:, 1, :], r=[sm_b], w=[sm_b])
                k.tt(abg[:, 2, :, :], ee[:], sm[:, 2, :].unsqueeze(2).to_broadcast([128, 8, 16]), ALU.mult,
                     r=[ee_b, sm_b], w=[abg_b])
                pre_steps(3)
                fw.op("dve", lambda: nc.vector.tensor_single_scalar(out=ci[:, 0], in_=ctop[:].bitcast(I32), scalar=255,
                                                                   op=ALU.bitwise_and), r=[ctop_b], w=[ci_b])
                fw.op("dve", lambda: nc.vector.tensor_single_scalar(out=ci[:, 1], in_=ci[:, 0], scalar=4,
                                                                   op=ALU.arith_shift_right), r=[ci_b], w=[ci_b])
                fw.op("dve", lambda: nc.vector.tensor_single_scalar(out=ci[:, 2], in_=ci[:, 0], scalar=15,
                                                                   op=ALU.bitwise_and), r=[ci_b], w=[ci_b])
                tcopy(cf[:], ci[:, 1:3], r=[ci_b], w=[cf_b])
                pre_steps(2)
                for w_ in range(2):
                    k.tt(cand[:].rearrange("p h (i j) -> p h i j", i=16), cf[:, w_].unsqueeze(3).to_broadcast([128, 8, 16, 16]),
                         iot[:, 0:16].unsqueeze(1).unsqueeze(1).to_broadcast([128, 8, 16, 16]), ALU.is_equal,
                         r=[cf_b, iot_b], w=[cand_b])
                    k.tt(cand[:].rearrange("p h (i j) -> p h i j", i=16), cand[:].rearrange("p h (i j) -> p h i j", i=16), idxv[:, :, w_, :].unsqueeze(2).to_broadcast([128, 8, 16, 16]), ALU.mult,
                         r=[cand_b, idxf_b], w=[cand_b])
                    k.red(abg[:, w_], cand[:].rearrange("p h (i j) -> p h i j", i=16), ALU.add, r=[cand_b], w=[abg_b])
                ptr, ptr_b = psb[4]
                for w_ in range(3):
                    k.tr(ptr[:, w_ * 128:(w_ + 1) * 128], abg[:, w_].rearrange("p h k -> p (h k)"), cmat[:, 0, :],
                         r=[abg_b, cmat_b], w=[ptr_b])
                aT_t, aT_b = abgT[t % 2]
                k.act(aT_t[:].rearrange("p a b -> p (a b)"), ptr[:, 0:384], AF.Copy, r=[ptr_b], w=[aT_b])
                ws_t, ws_b = Wst[0]
                TG = 8
                tcopy(aTb[:], aT_t[:, 0, :], r=[aT_b], w=[aTb_b])
                pend7 = []
                LAG = 2
                for g8 in range(128 // TG):
                    tk0 = g8 * TG
                    A_t, A_b = At[noh % NB_OH]
                    G_t, G_b = gGt[noh % NB_OH]
                    noh += 1
                    k.tt(A_t[:], iob16[:].unsqueeze(1).to_broadcast([128, TG, 128]),
                         aTb[:, tk0:tk0 + TG].unsqueeze(2).to_broadcast([128, TG, 128]), ALU.is_equal,
                         r=[iob_b, aTb_b], w=[A_b])
                    for qq in range(TG):
                        k.ts(G_t[:, qq, :], iob16[:], aT_t[:, 1, tk0 + qq:tk0 + qq + 1], aT_t[:, 2, tk0 + qq:tk0 + qq + 1],
                             ALU.is_equal, ALU.mult, r=[iob_b, aT_b], w=[G_b])

                    def wmm(A_t=A_t, A_b=A_b, G_t=G_t, G_b=G_b, tk0=tk0):
                        nonlocal_npw = pwctr
                        for q4 in range(TG // 4):
                            pw, pw_b = pwb[nonlocal_npw[0] % 2]
                            nonlocal_npw[0] += 1
                            for qq in range(4):
                                k.mm(pw[:, qq * 128:(qq + 1) * 128], G_t[:, q4 * 4 + qq, :], A_t[:, q4 * 4 + qq, :], True, True,
                                     r=[A_b, G_b], w=[pw_b])
                            k.act(ws_t[:, :, tk0 + q4 * 4:tk0 + (q4 + 1) * 4], pw[:].rearrange("p (t c) -> p c t", t=4), AF.Copy,
                                  r=[pw_b], w=[ws_b])
                    pend7.append(wmm)
                    if len(pend7) > LAG:
                        pend7.pop(0)()
                    pre_steps(2 if g8 == 0 else 1)
                    if g8 == 11 and t < 15:
                        load_qp(t + 1)
                        for hp in range(8):
                            score_mm(hp, None)
                        pre_issued = True
                while pend7:
                    pend7.pop(0)()
                k.ld(W_dv[:, :, t * 128:(t + 1) * 128], ws_t[:], ws_b, r=[ws_b], w=[W_b])
        for _ in pre_gen:
            pass
        if debug:
            da, da_b = k.dscr("dbg_abgT", [128, 3, 128], F32)
            k.ld(da, abgT[1][0][:], abgT[1][1], r=[abgT[1][1]], w=[da_b])
            dbgb.append(da_b)
        end_phase()
    if stop_after <= 7:
        return finish()

    with ExitStack() as ph:
        acc_bufs = [Buf("acc%d" % i) for i in range(4)]
        grow, grow_b = load_grow(ph, "grow8")
        acc, acc_b = sbt(ph, "acc", [128, 4, D], F32)
        GC = 8
        uG = [sbt(ph, "uG%d" % i, [128, GC, 512], BF16) for i in range(2)]
        vG = [sbt(ph, "vG%d" % i, [128, GC, D], BF16) for i in range(2)]
        WG = [sbt(ph, "WG%d" % i, [128, GC, 512], BF16) for i in range(2)]
        actT = [sbt(ph, "actT%d" % i, [128, GC, 512], BF16) for i in range(2)]
        x1t = [sbt(ph, "p8x%d" % i, [128, D], F32) for i in range(2)]
        fst = [sbt(ph, "fst%d" % i, [128, 2], F32) for i in range(2)]
        junk, junk_b = sbt(ph, "p8junk", [128, D], BF16)
        gi = 0
        for tb in range(4):
            tbs = slice(tb * 512, (tb + 1) * 512)
            for G in range(128 // GC):
                c0 = G * GC
                u_t, u_b = uG[gi % 2]
                v_t, v_b2 = vG[gi % 2]
                w_t, w_b = WG[gi % 2]
                a_t, a_b = actT[gi % 2]
                gi += 1
                for hv in range(GC // 4):
                    k.ldc(v_t[:, hv * 4:(hv + 1) * 4, :], vP[c0 + hv * 4:c0 + (hv + 1) * 4].rearrange("c i d -> i c d"), v_b2, w=[v_b2])
                k.ld(w_t[:], W_dv[:, c0:c0 + GC, tbs], w_b, r=[W_b], w=[w_b])
                k.ld(u_t[:], ga_d.rearrange("c i t -> i c t")[:, c0:c0 + GC, tbs], u_b, r=[ga_b], w=[u_b])
                for hv in range(GC // 4):
                    k.tt(a_t[:, hv * 4:(hv + 1) * 4, :], u_t[:, hv * 4:(hv + 1) * 4, :], w_t[:, hv * 4:(hv + 1) * 4, :], ALU.mult,
                         r=[u_b, w_b], w=[a_b])
                for tl in range(4):
                    ts_ = slice(tl * 128, (tl + 1) * 128)
                    for nb in range(4):
                        ns = slice(nb * 512, (nb + 1) * 512)
                        po, po_b = psb[(4 * (tl % 2) + nb)]
                        for cc in range(GC):
                            k.mm(po[:], a_t[:, cc, ts_], v_t[:, cc, ns], cc == 0, cc == GC - 1, r=[a_b, v_b2], w=[po_b])
                        if G == 0:
                            k.act(acc[:, tl, ns], po[:], AF.Copy, r=[po_b], w=[acc_bufs[tl]])
                        else:
                            k.tt(acc[:, tl, ns], acc[:, tl, ns], po[:], ALU.add, r=[acc_bufs[tl], po_b], w=[acc_bufs[tl]])
            for tl in range(4):
                t = tb * 4 + tl
                x_t, x_b = x1t[t % 2]
                s_t, s_b2 = fst[t % 2]
                k.ld(x_t[:], x1_d[t * 128:(t + 1) * 128, :], x_b, r=[x1_b], w=[x_b])
                k.tt(acc[:, tl, :], acc[:, tl, :], grow[:, 1, :], ALU.mult, r=[acc_bufs[tl], grow_b], w=[acc_bufs[tl]])
                k.tt(x_t[:], x_t[:], acc[:, tl, :], ALU.add, r=[x_b, acc_bufs[tl]], w=[x_b])
                k.act(junk[:], x_t[:], AF.Square, r=[x_b], w=[junk_b, s_b2], accum_out=s_t[:, 0:1])
                k.act(s_t[:, 1:2], s_t[:, 0:1], AF.Sqrt, r=[s_b2], w=[s_b2], scale=1.0 / D, bias=EPS)
                k.recip(s_t[:, 1:2], s_t[:, 1:2], r=[s_b2], w=[s_b2])
                k.ts(x_t[:], x_t[:], s_t[:, 1:2], None, ALU.mult, None, r=[x_b, s_b2], w=[x_b])
                k.tt(x_t[:], x_t[:], grow[:, 2, :], ALU.mult, r=[x_b, grow_b], w=[x_b])
                k.ld(out_d[t * 128:(t + 1) * 128, :], x_t[:], x_b, r=[x_b], w=[out_b])
        end_phase()
    return finish()


def _rope_tables(pos_list):
    pos = np.asarray(pos_list)
    n_freq = 32
    inv = (np.float32(10000.0) ** (-np.arange(n_freq, dtype=np.float32) / np.float32(n_freq))).astype(np.float32)
    p = np.maximum(pos, 0)
    row = (p // 64).astype(np.float32)
    col = (p % 64).astype(np.float32)
    ang = np.concatenate([row[:, None] * inv[None, :], col[:, None] * inv[None, :]], axis=1).astype(np.float32)
    cos = np.cos(ang).astype(np.float32)
    sin = np.sin(ang).astype(np.float32)
    isctx = pos < 0
    cos[isctx] = 1.0
    sin[isctx] = 0.0
    cosT = np.concatenate([cos, cos], axis=1).T
    sinS = np.concatenate([-sin, sin], axis=1).T
    return np.ascontiguousarray(cosT, dtype=np.float32), np.ascontiguousarray(sinS, dtype=np.float32)


def _consts():
    p = np.arange(128)
    ident = np.eye(128, dtype=np.float32)
    pswap = np.zeros((128, 128), np.float32)
    pswap[(p + 64) % 128, p] = 1.0
    kk, qq = np.meshgrid(p, p, indexing="ij")
    D1 = np.maximum(qq - kk, 0).astype(np.float32)
    U1 = (qq >= kk).astype(np.float32)
    D2 = np.maximum(kk - qq, 0).astype(np.float32)
    U2 = (kk >= qq).astype(np.float32)
    POSF = np.tile((p + 1).astype(np.float32)[None, :], (128, 1))
    POSB = np.tile((128 - p).astype(np.float32)[None, :], (128, 1))
    cmat = np.stack([ident, pswap, D1, U1, D2, U2, POSF, POSB], axis=1)
    kpos = np.concatenate([np.tile((127 - p).astype(np.float32)[:, None], (1, 8)),
                           np.tile(p.astype(np.float32)[:, None], (1, 8))], axis=1)
    n = (np.arange(16)[None, :] * 128 + p[:, None]).astype(np.float32)
    npos = np.stack([-(n - 1024.0), n - 1023.0], axis=1).astype(np.float32)
    nn = np.arange(2048, dtype=np.float32)
    qpos = np.stack([np.tile((nn - 1024.0)[None, :], (128, 1)), np.tile((1023.0 - nn)[None, :], (128, 1))], axis=1).astype(np.float32)
    iot = np.zeros((128, 512), np.float32)
    iot[:, 0:128] = np.arange(128, dtype=np.float32)[None, :]
    iot[:, 128:384] = np.arange(256, dtype=np.int32).view(np.float32)[None, :]
    iot[:, 384:512] = np.arange(128, dtype=np.int32).view(np.float32)[None, :]
    return np.ascontiguousarray(cmat), np.ascontiguousarray(kpos), iot, np.ascontiguousarray(npos), np.ascontiguousarray(qpos)


def _dist(half):
    BIG = np.float32(1e6)
    d = np.zeros((128, 2, NT_ALL), np.float32)
    p = np.arange(128, dtype=np.float32)
    for t in range(2):
        c = t * 128 + p
        d[:, 0, t] = (255 - c) + (2048 if half == 1 else 0) + 1025
        d[:, 1, t] = c + (2048 if half == 0 else 0) + 1025
    for t in range(2, 18):
        loc = (t - 2) * 128 + p
        if half == 1:
            d[:, 0, t] = 2047 - loc + 1025
            d[:, 1, t] = BIG
        else:
            d[:, 0, t] = BIG
            d[:, 1, t] = loc + 1025
    return d


def prep(inputs, cores=range(8)):
    f = lambda a: np.ascontiguousarray(np.asarray(a), dtype=np.float32)
    x = f(inputs["x"]); c = f(inputs["c"]); ctx = f(inputs["ctx"]); c_ctx = f(inputs["c_ctx"])
    cmat, kpos, iot, npos, qpos = _consts()
    shared = {
        "ada_w": f(inputs["ada_w"])[0],
        "ada_bT": np.ascontiguousarray(f(inputs["ada_b"])[0].reshape(96, 128).T),
        "n1w": np.ascontiguousarray(f(inputs["norm1_w"])[0].reshape(16, 128).T),
        "n2w": np.ascontiguousarray(f(inputs["norm2_w"])[0].reshape(16, 128).T),
        "nfw": f(inputs["norm_f_w"]),
        "w_in": f(inputs["w_in"])[0],
        "qkw": np.ascontiguousarray(np.stack([f(inputs["q_norm_w"])[0], f(inputs["k_norm_w"])[0]], axis=1)),
        "zdec": np.concatenate([f(inputs["ret_decay_fwd"])[0], f(inputs["ret_decay_bwd"])[0]]),
        "rnw": f(inputs["ret_norm_w"])[0],
        "w_ab": f(inputs["w_attn_branch"])[0],
        "w_rb": f(inputs["w_ret_branch"])[0],
        "w_mo": f(inputs["w_merge_out"])[0],
        "pwq": f(inputs["peer_w_q"])[0],
        "keysT": np.ascontiguousarray(f(inputs["peer_keys"])[0].reshape(16, 128, 128).transpose(2, 0, 1)),
        "uT": np.ascontiguousarray(f(inputs["peer_u"])[0].reshape(128, 128, D).transpose(2, 0, 1)),
        "vP": f(inputs["peer_v"])[0].reshape(128, 128, D),
        "cmat": cmat, "kpos": kpos, "iotas": iot, "npos": npos, "qpos": qpos,
    }
    maps = []
    for core in cores:
        b, half = core // 2, core % 2
        own = np.arange(half * 2048, (half + 1) * 2048)
        oth = np.arange((1 - half) * 2048, (2 - half) * 2048)
        pos = np.concatenate([-np.ones(256, np.int64), oth, own])
        cosT, sinS = _rope_tables(pos)
        m = dict(shared)
        m["tok"] = np.ascontiguousarray(np.concatenate([ctx[b], x[b][oth], x[b][own]], axis=0))
        m["cT"] = np.ascontiguousarray(np.stack([c[b].reshape(16, 128).T, c_ctx.reshape(16, 128).T], axis=2))
        m["cosT"] = cosT
        m["sinS"] = sinS
        m["dist"] = _dist(half)
        maps.append(m)
    return maps


_NC_CACHE = {}


def kernel(**inputs):
    if "nc" not in _NC_CACHE:
        _NC_CACHE["nc"] = build()
    nc = _NC_CACHE["nc"]
    maps = prep(inputs)
    res = run_bass_kernel_spmd(nc, maps, core_ids=list(range(8)))
    out = np.zeros((4, SEQ, D), np.float32)
    for core in range(8):
        b, half = core // 2, core % 2
        out[b, half * 2048:(half + 1) * 2048, :] = np.asarray(res.results[core]["out"], dtype=np.float32)
    return out
```
